# Optimizing a Trainium2 kernel written in Bass

```python
import jax, jax.numpy as jnp
from jax import lax
import numpy as np


D_MODEL = 1024
BATCH = 8
SEQ = 2048
DEPTH = 2

MLA_HEADS = 8
MLA_NOPE = 64
MLA_ROPE = 32
MLA_V = 64
Q_LORA = 384
KV_LORA = 256
ROPE_BASE = 10000.0
DSA_HEADS = 8
DSA_DIM = 64
IDX_HEADS = 8
IDX_DIM = 32
TOPK_MAX = 256
D_MIX = MLA_HEADS * MLA_V + DSA_HEADS * DSA_DIM
N_EXPERTS = 16
N_GROUPS = 4
EXPERTS_PER_GROUP = N_EXPERTS // N_GROUPS
TOP_K_EXPERTS = 2
GROUP_SCORE_K = 2
D_FF_EXPERT = 512
ALPHA = (2.0 * DEPTH) ** 0.25
BETA = (8.0 * DEPTH) ** -0.25
Q_BLOCK = 128
LN_EPS = 1e-5
RMS_EPS = 1e-6

SPLIT_SIZES = (Q_LORA, KV_LORA, MLA_ROPE, DSA_HEADS * DSA_DIM, DSA_DIM, DSA_DIM,
               IDX_HEADS * IDX_DIM, IDX_DIM, IDX_HEADS)
SPLIT_POINTS = tuple(int(v) for v in np.cumsum(SPLIT_SIZES)[:-1])
IN_COLS = int(sum(SPLIT_SIZES))
DSA_V_START = int(sum(SPLIT_SIZES[:5]))

kernel_name = 'hybrid_mla_dsa_grouped_moe_deepnorm'


def layer_norm(x, g, b):
    xf = x.astype(jnp.float32)
    mu = jnp.mean(xf, axis=-1, keepdims=True)
    var = jnp.mean(jnp.square(xf - mu), axis=-1, keepdims=True)
    y = (xf - mu) * lax.rsqrt(var + LN_EPS) * g.astype(jnp.float32) + b.astype(jnp.float32)
    return y.astype(x.dtype)


def rms_norm(x, g):
    xf = x.astype(jnp.float32)
    y = xf * lax.rsqrt(jnp.mean(jnp.square(xf), axis=-1, keepdims=True) + RMS_EPS)
    return (y * g.astype(jnp.float32)).astype(x.dtype)


def rope(x, pos):
    half = x.shape[-1] // 2
    inv = ROPE_BASE ** (-jnp.arange(half, dtype=jnp.float32) / half)
    ang = pos[:, None] * inv[None, :]
    cos = jnp.cos(ang)[None, :, None, :]
    sin = jnp.sin(ang)[None, :, None, :]
    xf = x.astype(jnp.float32)
    x1, x2 = xf[..., :half], xf[..., half:]
    return jnp.concatenate([x1 * cos - x2 * sin, x1 * sin + x2 * cos], axis=-1).astype(x.dtype)


def sweep_query_blocks(block_fn, seq_len):
    starts = jnp.arange(seq_len // Q_BLOCK, dtype=jnp.int32) * Q_BLOCK
    out = lax.map(block_fn, starts)
    nb, b, qb, h, d = out.shape
    return jnp.transpose(out, (1, 0, 2, 3, 4)).reshape(b, nb * qb, h * d)


def mla_group(qa, kva, kr, q_norm_g, w_q_up, kv_norm_g, w_uk, w_uv, pos):
    B, T, _ = qa.shape
    q = (rms_norm(qa, q_norm_g) @ w_q_up).reshape(B, T, MLA_HEADS, MLA_NOPE + MLA_ROPE)
    q = jnp.concatenate([q[..., :MLA_NOPE], rope(q[..., MLA_NOPE:], pos)], axis=-1)
    c_kv = rms_norm(kva, kv_norm_g)
    k_nope = (c_kv @ w_uk).reshape(B, T, MLA_HEADS, MLA_NOPE)
    v = (c_kv @ w_uv).reshape(B, T, MLA_HEADS, MLA_V)
    k_rope = rope(kr[:, :, None, :], pos)
    k = jnp.concatenate([k_nope, jnp.broadcast_to(k_rope, (B, T, MLA_HEADS, MLA_ROPE))], axis=-1)
    scale = (MLA_NOPE + MLA_ROPE) ** -0.5
    key_idx = jnp.arange(T, dtype=jnp.int32)

    def block(start):
        qb = lax.dynamic_slice_in_dim(q, start, Q_BLOCK, axis=1)
        s = jnp.einsum('bthd,bshd->bhts', qb, k).astype(jnp.float32) * scale
        tq = start + jnp.arange(Q_BLOCK, dtype=jnp.int32)
        s = jnp.where(key_idx[None, :] <= tq[:, None], s, -jnp.inf)
        p = jax.nn.softmax(s, axis=-1).astype(v.dtype)
        return jnp.einsum('bhts,bshd->bthd', p, v)

    return sweep_query_blocks(block, T)


def dsa_group(dq, dk, dv, iq, ik, iw, slopes, n_keep):
    B, T, _ = dq.shape
    q = dq.reshape(B, T, DSA_HEADS, DSA_DIM)
    iq = iq.reshape(B, T, IDX_HEADS, IDX_DIM)
    iw = iw * (IDX_HEADS ** -0.5)
    key_idx = jnp.arange(T, dtype=jnp.int32)
    scale = DSA_DIM ** -0.5
    gather = jax.vmap(lambda arr, idx: arr[idx])

    def block(start):
        qb = lax.dynamic_slice_in_dim(q, start, Q_BLOCK, axis=1)
        iqb = lax.dynamic_slice_in_dim(iq, start, Q_BLOCK, axis=1)
        iwb = lax.dynamic_slice_in_dim(iw, start, Q_BLOCK, axis=1)
        tq = start + jnp.arange(Q_BLOCK, dtype=jnp.int32)
        rel = jax.nn.relu(jnp.einsum('bthd,bsd->bths', iqb, ik) * (IDX_DIM ** -0.5))
        score = jnp.einsum('bths,bth->bts', rel, iwb).astype(jnp.float32)
        score = jnp.where(key_idx[None, None, :] <= tq[None, :, None], score, -jnp.inf)
        _, sel = lax.top_k(score, n_keep)
        k_sel = gather(dk, sel)
        v_sel = gather(dv, sel)
        dist = (tq[None, :, None] - sel).astype(jnp.float32)
        s = jnp.einsum('bthd,btkd->bhtk', qb, k_sel).astype(jnp.float32) * scale
        s = s - slopes[None, :, None, None] * dist[:, None, :, :]
        s = jnp.where((dist >= 0.0)[:, None, :, :], s, -jnp.inf)
        p = jax.nn.softmax(s, axis=-1).astype(v_sel.dtype)
        return jnp.einsum('bhtk,btkd->bthd', p, v_sel)

    return sweep_query_blocks(block, T)


def hybrid_mixer(h, w_in, q_norm_g, w_q_up, kv_norm_g, w_uk, w_uv, w_o, pos, slopes, n_keep):
    proj = h @ w_in
    qa, kva, kr, dq, dk, dv, iq, ik, iw = jnp.split(proj, SPLIT_POINTS, axis=-1)
    out_a = mla_group(qa, kva, kr, q_norm_g, w_q_up, kv_norm_g, w_uk, w_uv, pos)
    out_b = dsa_group(dq, dk, dv, iq, ik, iw, slopes, n_keep)
    return jnp.concatenate([out_a, out_b], axis=-1) @ w_o


def grouped_moe(h, router_w, router_bias, w_gate, w_up, w_down):
    B, T, D = h.shape
    xt = h.reshape(B * T, D)
    aff = jax.nn.sigmoid((xt @ router_w).astype(jnp.float32))
    biased = aff + router_bias.astype(jnp.float32)
    grouped = biased.reshape(-1, N_GROUPS, EXPERTS_PER_GROUP)
    group_score = jnp.sum(lax.top_k(grouped, GROUP_SCORE_K)[0], axis=-1)
    g_sel = jnp.argmax(group_score, axis=-1)
    in_group = (jnp.arange(N_EXPERTS) // EXPERTS_PER_GROUP)[None, :] == g_sel[:, None]
    _, e_idx = lax.top_k(jnp.where(in_group, biased, -jnp.inf), TOP_K_EXPERTS)
    wts = jnp.take_along_axis(aff, e_idx, axis=-1)
    wts = wts / jnp.sum(wts, axis=-1, keepdims=True)
    gates = jnp.sum(jax.nn.one_hot(e_idx, N_EXPERTS, dtype=jnp.float32) * wts[..., None], axis=1)
    hg = jnp.einsum('nd,edf->nef', xt, w_gate)
    hu = jnp.einsum('nd,edf->nef', xt, w_up)
    act = jax.nn.silu(hg) * hu * gates[:, :, None].astype(hg.dtype)
    out = jnp.einsum('nef,efd->nd', act, w_down)
    return out.reshape(B, T, D)


def setup_inputs(seed: int = 0) -> dict:
    key = jax.random.key(seed)
    ks = jax.random.split(key, 20)
    f32 = jnp.float32
    nrm = lambda k, shape, s: jax.random.normal(k, shape, f32) * s
    col_scale = np.ones((IN_COLS,), np.float32)
    col_scale[DSA_V_START:DSA_V_START + DSA_DIM] = BETA
    w_in = nrm(ks[1], (DEPTH, D_MODEL, IN_COLS), D_MODEL ** -0.5) * jnp.asarray(col_scale)
    return {
        'x': jax.random.normal(ks[0], (BATCH, SEQ, D_MODEL), f32),
        'w_in': w_in,
        'q_norm_g': 1.0 + nrm(ks[2], (DEPTH, Q_LORA), 0.02),
        'w_q_up': nrm(ks[3], (DEPTH, Q_LORA, MLA_HEADS * (MLA_NOPE + MLA_ROPE)), Q_LORA ** -0.5),
        'kv_norm_g': 1.0 + nrm(ks[4], (DEPTH, KV_LORA), 0.02),
        'w_uk': nrm(ks[5], (DEPTH, KV_LORA, MLA_HEADS * MLA_NOPE), KV_LORA ** -0.5),
        'w_uv': nrm(ks[6], (DEPTH, KV_LORA, MLA_HEADS * MLA_V), BETA * KV_LORA ** -0.5),
        'w_o': nrm(ks[7], (DEPTH, D_MIX, D_MODEL), BETA * D_MIX ** -0.5),
        'ln1_g': 1.0 + nrm(ks[8], (DEPTH, D_MODEL), 0.02),
        'ln1_b': nrm(ks[9], (DEPTH, D_MODEL), 0.02),
        'router_w': nrm(ks[10], (D_MODEL, N_EXPERTS), D_MODEL ** -0.5),
        'router_bias': nrm(ks[11], (N_EXPERTS,), 0.01),
        'w_gate': nrm(ks[12], (DEPTH, N_EXPERTS, D_MODEL, D_FF_EXPERT), BETA * D_MODEL ** -0.5),
        'w_up': nrm(ks[13], (DEPTH, N_EXPERTS, D_MODEL, D_FF_EXPERT), BETA * D_MODEL ** -0.5),
        'w_down': nrm(ks[14], (DEPTH, N_EXPERTS, D_FF_EXPERT, D_MODEL), BETA * D_FF_EXPERT ** -0.5),
        'ln2_g': 1.0 + nrm(ks[15], (DEPTH, D_MODEL), 0.02),
        'ln2_b': nrm(ks[16], (DEPTH, D_MODEL), 0.02),
    }


def reference(x, w_in, q_norm_g, w_q_up, kv_norm_g, w_uk, w_uv, w_o, ln1_g, ln1_b,
              router_w, router_bias, w_gate, w_up, w_down, ln2_g, ln2_b):
    T = x.shape[1]
    pos = jnp.arange(T, dtype=jnp.float32)
    n_keep = min(TOPK_MAX, T // 4)
    slopes = 2.0 ** (-8.0 * jnp.arange(1, DSA_HEADS + 1, dtype=jnp.float32) / DSA_HEADS)
    for l in range(DEPTH):
        mix = hybrid_mixer(x, w_in[l], q_norm_g[l], w_q_up[l], kv_norm_g[l], w_uk[l], w_uv[l],
                           w_o[l], pos, slopes, n_keep)
        x = layer_norm(ALPHA * x + mix, ln1_g[l], ln1_b[l])
        ffn = grouped_moe(x, router_w, router_bias, w_gate[l], w_up[l], w_down[l])
        x = layer_norm(ALPHA * x + ffn, ln2_g[l], ln2_b[l])
    return x
```

```python
from contextlib import ExitStack
import numpy as np
import ml_dtypes
import concourse.bass as bass
import concourse.mybir as mybir
from concourse.bass_utils import run_bass_kernel_spmd

F32 = mybir.dt.float32
BF16 = mybir.dt.bfloat16
AF = mybir.ActivationFunctionType
ALU = mybir.AluOpType
AX = mybir.AxisListType

T = 2048
D = 1024
DEPTH = 2
NT = 16
NE = 16
DFF = 512
ALPHA = (2.0 * DEPTH) ** 0.25
LN_EPS = 1e-5
RMS_EPS = 1e-6
MLA_SCALE = 96.0 ** -0.5
NBIS = 16
BIG = 1.0e30
ENG_NAMES = ("pe", "act", "dve", "pool", "sp")


class Prog:
    def __init__(self, nc, st):
        self.nc = nc
        self.st = st
        self.esem = {e: st.enter_context(nc.semaphore("s_" + e)) for e in ENG_NAMES}
        self.ecount = {e: 0 for e in ENG_NAMES}
        self.csem = {}
        self.ccount = {}
        self.reset()

    def reset(self):
        self.ops = []
        self.res = {}

    def chan(self, name):
        if name not in self.csem:
            self.csem[name] = self.st.enter_context(self.nc.semaphore("c_" + name))
            self.ccount[name] = 0
        return name

    EXCL = ("pa", "pb", "tpb", "pc", "tp", "ST", "OB", "GA", "GB", "BCP", "TPM", "YP", "LGP", "HG", "HU", "YO")

    def op(self, eng, fn, r=(), w=(), chan=None):
        idx = len(self.ops)
        w = list(w) + [k for k in r if isinstance(k, str) and k.startswith(self.EXCL) and k not in w]
        deps = set()
        for k in r:
            e = self.res.get(k)
            if e is not None and e[0] is not None:
                deps.add(e[0])
        for k in w:
            e = self.res.get(k)
            if e is not None:
                if e[0] is not None:
                    deps.add(e[0])
                deps.update(e[1])
        deps.discard(idx)
        for k in r:
            e = self.res.setdefault(k, [None, []])
            e[1].append(idx)
        for k in w:
            self.res[k] = [idx, []]
        last = {}
        keep = set()
        for d in deps:
            o = self.ops[d]
            if o["chan"] is not None:
                keep.add(d)
            else:
                last[o["eng"]] = max(last.get(o["eng"], -1), d)
        deps = keep | set(last.values())
        dl = []
        for d in deps:
            o = self.ops[d]
            if o["chan"] is not None:
                dl.append(("c", o["chan"], self.ccount[o["chan"]]))
            else:
                dl.append(("e", d))
        cval = None
        if chan is not None:
            self.chan(chan)
            self.ccount[chan] += 16
            cval = self.ccount[chan]
        self.ops.append(dict(eng=eng, fn=fn, deps=dl, chan=chan, cval=cval, sig=False, sval=None))
        return idx

    def emit(self, final_wait=True):
        import os
        self.phase_idx = getattr(self, "phase_idx", -1) + 1
        skip = os.environ.get("SKIP_PHASES", "")
        if skip and str(self.phase_idx) in skip.split(","):
            self.reset()
            return
        nc = self.nc
        ops = self.ops
        for o in ops:
            for d in o["deps"]:
                if d[0] == "e":
                    p = ops[d[1]]
                    if p["eng"] != o["eng"] or o["eng"] != "pe" or o["chan"] is not None:
                        p["sig"] = True
        for o in ops:
            if o["chan"] is None and o["sig"]:
                self.ecount[o["eng"]] += 1
                o["sval"] = self.ecount[o["eng"]]
        per = {e: [] for e in ENG_NAMES}
        for o in ops:
            per[o["eng"]].append(o)
        chans_by_eng = {e: {} for e in ENG_NAMES}
        for o in ops:
            if o["chan"] is not None:
                chans_by_eng[o["eng"]][o["chan"]] = o["cval"]

        def replay(ename, eng):
            waited = {}
            for o in per[ename]:
                for d in o["deps"]:
                    if d[0] == "c":
                        sem, val, key = self.csem[d[1]], d[2], ("c", d[1])
                    else:
                        p = ops[d[1]]
                        if not p["sig"]:
                            continue
                        sem, val, key = self.esem[p["eng"]], p["sval"], ("e", p["eng"])
                    if waited.get(key, -1) >= val:
                        continue
                    waited[key] = val
                    eng.wait_ge(sem, val)
                ins = o["fn"](eng)
                if o["chan"] is not None:
                    ins.then_inc(self.csem[o["chan"]], 16)
                elif o["sig"]:
                    ins.then_inc(self.esem[ename], 1)
            if final_wait:
                for c, v in chans_by_eng[ename].items():
                    eng.wait_ge(self.csem[c], v)

        with nc.Block() as block:
            block.tensor(lambda e: replay("pe", e))
            block.scalar(lambda e: replay("act", e))
            block.vector(lambda e: replay("dve", e))
            block.gpsimd(lambda e: replay("pool", e))
            block.sync(lambda e: replay("sp", e))
        self.reset()


def sl(i, n=128):
    return slice(i * n, (i + 1) * n)


class TickQ:
    def __init__(self):
        self.q = []

    def push(self, delay, fn):
        self.q.append([delay, fn])

    def tick(self):
        for e in self.q:
            e[0] -= 1
        due = [e for e in self.q if e[0] <= 0]
        self.q = [e for e in self.q if e[0] > 0]
        for e in due:
            e[1]()

    def flush(self):
        while self.q:
            self.tick()


def emit_layernorm(P, pfx, ys, xo, gb, bb, st6, mv, key_in, key_out):
    P.op("dve", lambda e: e.bn_stats(out=st6[:, 0:6], in_=ys[:, 0:512]), r=[key_in], w=[pfx + "st6a"])
    P.op("dve", lambda e: e.bn_stats(out=st6[:, 6:12], in_=ys[:, 512:1024]), r=[key_in], w=[pfx + "st6b"])
    P.op("dve", lambda e: e.bn_aggr(out=mv[:, 0:2], in_=st6[:, 0:12]), r=[pfx + "st6a", pfx + "st6b"], w=[pfx + "mv"])
    P.op("dve", lambda e: e.tensor_scalar(out=mv[:, 2:3], in0=mv[:, 1:2], scalar1=LN_EPS, scalar2=None, op0=ALU.add),
         r=[pfx + "mv"], w=[pfx + "mv2"])
    P.op("act", lambda e: e.activation(out=mv[:, 3:4], in_=mv[:, 2:3], func=AF.Ln), r=[pfx + "mv2"], w=[pfx + "mv3"])
    P.op("act", lambda e: e.activation(out=mv[:, 4:5], in_=mv[:, 3:4], func=AF.Exp, scale=-0.5), r=[pfx + "mv3"], w=[pfx + "mv4"])
    P.op("dve", lambda e: e.tensor_scalar(out=ys[:, :], in0=ys[:, :], scalar1=mv[:, 0:1], scalar2=mv[:, 4:5],
                                         op0=ALU.subtract, op1=ALU.mult),
         r=[key_in, pfx + "mv", pfx + "mv4"], w=[key_in])
    P.op("pool", lambda e: e.tensor_tensor(out=ys[:, :], in0=ys[:, :], in1=gb[:, :], op=ALU.mult),
         r=[key_in, "lng"], w=[key_in])
    P.op("pool", lambda e: e.tensor_tensor(out=xo[:, :], in0=ys[:, :], in1=bb[:, :], op=ALU.add),
         r=[key_in, "lnb"], w=[key_out])


def emit_xt_update(P, xo, key_x, XT, tt, identf, tp0, tp1, extra=None):
    for half, tp in ((0, tp0), (1, tp1)):
        kb = "tp%d" % half
        for q in range(4):
            c = half * 4 + q
            P.op("pe", lambda e, c=c, q=q, tp=tp: e.transpose(out=tp[:, sl(q)], in_=xo[:, sl(c)], identity=identf[:, :]),
                 r=[key_x, "identf"], w=[kb])
        P.op("act", lambda e, half=half, tp=tp: e.activation(
            out=XT[:, half * 4:half * 4 + 4, sl(tt)], in_=tp[:, :].rearrange("p (c t) -> p c t", c=4), func=AF.Copy),
            r=[kb], w=[("XT", tt)])
        if extra is not None:
            extra(half, tp, kb)


def build_program(n_layers=DEPTH, stop_after=None, taps=None):
    nc = bass.Bass("TRN2", target_bir_lowering=False)
    dt_in = {}

    def din(name, shape, dt=F32):
        dt_in[name] = nc.dram_tensor(name, list(shape), dt, kind="ExternalInput").ap()
        return dt_in[name]

    x = din("x", [T, D])
    w_in = din("w_in", [DEPTH, D, 1608])
    q_norm_g = din("q_norm_g", [DEPTH, 384])
    w_q_up = din("w_q_up", [DEPTH, 384, 768])
    kv_norm_g = din("kv_norm_g", [DEPTH, 256])
    w_uk = din("w_uk", [DEPTH, 256, 512])
    w_uv = din("w_uv", [DEPTH, 256, 512])
    w_o = din("w_o", [DEPTH, D, D])
    ln1_g = din("ln1_g", [DEPTH, D])
    ln1_b = din("ln1_b", [DEPTH, D])
    router_w = din("router_w", [D, NE])
    router_bias = din("router_bias", [1, NE])
    w_gate = din("w_gate", [DEPTH, NE, D, DFF])
    w_up = din("w_up", [DEPTH, NE, D, DFF])
    w_down = din("w_down", [DEPTH, NE, DFF, D])
    ln2_g = din("ln2_g", [DEPTH, D])
    ln2_b = din("ln2_b", [DEPTH, D])
    c_identf = din("c_identf", [128, 128])
    c_identb = din("c_identb", [128, 128], BF16)
    c_cos = din("c_cos", [32, T])
    c_sin = din("c_sin", [32, T])
    c_tri = din("c_tri", [128, 128], BF16)
    c_triadd = din("c_triadd", [128, 128])
    c_alq = din("c_alq", [4, T], BF16)
    c_alk = din("c_alk", [4, T], BF16)

    out = nc.dram_tensor("out", [T, D], F32, kind="ExternalOutput").ap()
    xs1 = nc.dram_tensor("xs1", [T, D], F32, kind="Internal").ap()
    xs2 = nc.dram_tensor("xs2", [T, D], F32, kind="Internal").ap()
    tap_aps = {}
    if taps:
        for k, (shp, dt) in taps.items():
            tap_aps[k] = nc.dram_tensor("tap_" + k, list(shp), dt, kind="ExternalOutput").ap()

    st = ExitStack()
    sfx = [""]
    with st:
        P = Prog(nc, st)
        sb = lambda name, shape, dt=F32: st.enter_context(nc.sbuf_tensor(name + sfx[0], list(shape), dt))
        XT = sb("XT", [128, 8, T], BF16)
        MIXT = sb("MIXT", [128, 8, T], BF16)
        identf = sb("identf", [128, 128])
        identb = sb("identb", [128, 128], BF16)
        ones_f = sb("ones_f", [128, 128])
        GATES = sb("GATES", [128, NT, NE])

        def tap(P_, name, src_ap, key):
            if name in tap_aps:
                P_.op("sp", lambda e: e.dma_start(out=tap_aps[name], in_=src_ap), r=list(key), w=["tap_" + name],
                      chan="tap_" + name)

        with ExitStack() as ph:
            psb = lambda name, shape, dt=F32: ph.enter_context(nc.sbuf_tensor(name + sfx[0], list(shape), dt))
            pps = lambda name, shape, dt=F32: ph.enter_context(nc.psum_tensor(name + sfx[0], list(shape), dt))
            xin = [psb("p0_xin%d" % i, [128, D]) for i in range(2)]
            tp0 = pps("p0_tp0", [128, 512])
            tp1 = pps("p0_tp1", [128, 512])
            P.op("sp", lambda e: e.dma_start(out=identf[:, :], in_=c_identf), w=["identf"], chan="identf")
            P.op("sp", lambda e: e.dma_start(out=identb[:, :], in_=c_identb), w=["identb"], chan="identb")
            P.op("pool", lambda e: e.memset(ones_f[:, :], 1.0), w=["ones_f"])
            for tt in range(NT):
                b = tt % 2
                kx = "xin%d" % b
                P.op("sp", lambda e, tt=tt, b=b: e.dma_start(out=xin[b][:, :], in_=x[sl(tt), :]), w=[kx], chan=kx)
                emit_xt_update(P, xin[b], kx, XT, tt, identf, tp0, tp1)
            if stop_after == "p0" and "XT" in tap_aps:
                for tt in range(NT):
                    P.op("sp", lambda e, tt=tt: e.dma_start(out=tap_aps["XT"][:, :, sl(tt)], in_=XT[:, :, sl(tt)]),
                         r=[("XT", tt)], w=["tapXT%d" % tt], chan="tapXT")
            P.emit()

        for l in range(n_layers):
            sfx[0] = "_L%d" % l
            xsrc = x if l == 0 else xs2
            xdst2 = out if l == n_layers - 1 else xs2
            if stop_after == "p0":
                break
            mla = ExitStack()
            msb = lambda name, shape, dt=F32: mla.enter_context(nc.sbuf_tensor(name + sfx[0], list(shape), dt))
            QANT = msb("QANT", [128, 3, T], BF16)
            CKVT = msb("CKVT", [128, 2, T], BF16)
            KRT = msb("KRT", [96, T], BF16)
            COS = msb("COS", [96, T])
            SIN = msb("SIN", [96, T])
            with ExitStack() as ph:
                psb = lambda name, shape, dt=F32: ph.enter_context(nc.sbuf_tensor(name + sfx[0], list(shape), dt))
                pps = lambda name, shape, dt=F32: ph.enter_context(nc.psum_tensor(name + sfx[0], list(shape), dt))
                WM = psb("WM", [128, 8, 672], BF16)
                KSW = psb("KSW", [128, 8, 96], BF16)
                junk = psb("mi_junk", [128, 384])
                ss = [psb("mi_ss%d" % i, [128, 8]) for i in range(2)]
                qn = [psb("mi_qn%d" % i, [128, 640], BF16) for i in range(2)]
                rt = psb("mi_rt", [96, 2, 512])
                PA = [pps("mi_pa%d" % i, [128, 512]) for i in range(2)]
                PB = [pps("mi_pb%d" % i, [128, 512]) for i in range(2)]
                TPB = [pps("mi_tp%d" % i, [128, 1024], BF16) for i in range(2)]
                PC = [pps("mi_pc%d" % i, [128, 512]) for i in range(2)]
                wl = w_in[l].rearrange("(c p) n -> p c n", p=128)
                for c in range(8):
                    P.op("pool", lambda e, c=c: e.dma_start(out=WM[:, c, :], in_=wl[:, c, 0:672]), w=["WM"], chan="WM")
                P.op("dve", lambda e: e.memset(KSW[:, :, :], 0.0), w=["KSW"])
                P.op("pool", lambda e: e.dma_start(out=KSW[:, :, 64:80], in_=wl[:, :, 656:672]), r=[], w=["KSW"], chan="KSW")
                P.op("pool", lambda e: e.dma_start(out=KSW[:, :, 80:96], in_=wl[:, :, 640:656]), r=[], w=["KSW"], chan="KSW")
                P.op("sp", lambda e: e.dma_start(out=COS[64:96, :], in_=c_cos), w=["COS"], chan="COS")
                P.op("sp", lambda e: e.dma_start(out=SIN[64:96, :], in_=c_sin), w=["SIN"], chan="SIN")
                miq = TickQ()

                def mi_tile(tt):
                    b = tt % 2
                    pa, pb, tpb, s_, qn_ = PA[b], PB[b], TPB[b], ss[b], qn[b]
                    ka, kb_, kt, ks, kq = "pa%d" % b, "pb%d" % b, "tpb%d" % b, "ss%d" % b, "qn%d" % b
                    for c in range(8):
                        P.op("pe", lambda e, c=c, pa=pa: e.matmul(pa[:, :], lhsT=XT[:, c, sl(tt)], rhs=WM[:, c, 0:512],
                                                                    start=(c == 0), stop=(c == 7)),
                             r=[("XT", tt), "WM"], w=[ka])
                    for c in range(8):
                        P.op("pe", lambda e, c=c, pb=pb: e.matmul(pb[:, 0:128], lhsT=XT[:, c, sl(tt)], rhs=WM[:, c, 512:640],
                                                                    start=(c == 0), stop=(c == 7)),
                             r=[("XT", tt), "WM"], w=[kb_])
                    P.op("act", lambda e, pa=pa, s_=s_: e.activation(out=junk[:, 0:384], in_=pa[:, 0:384], func=AF.Square,
                                                                     accum_out=s_[:, 0:1]), r=[ka], w=[ks + "a", "junk"])
                    P.op("act", lambda e, pa=pa, s_=s_: e.activation(out=junk[:, 0:128], in_=pa[:, 384:512], func=AF.Square,
                                                                     accum_out=s_[:, 1:2]), r=[ka], w=[ks + "b", "junk"])
                    P.op("act", lambda e, pb=pb, s_=s_: e.activation(out=junk[:, 0:128], in_=pb[:, 0:128], func=AF.Square,
                                                                     accum_out=s_[:, 2:3]), r=[kb_], w=[ks + "c", "junk"])
                    P.op("dve", lambda e, s_=s_: e.tensor_scalar(out=s_[:, 3:4], in0=s_[:, 0:1], scalar1=1.0 / 384, scalar2=RMS_EPS,
                                                                 op0=ALU.mult, op1=ALU.add), r=[ks + "a"], w=[ks + "d"])
                    P.op("dve", lambda e, s_=s_: e.tensor_tensor(out=s_[:, 1:2], in0=s_[:, 1:2], in1=s_[:, 2:3], op=ALU.add),
                         r=[ks + "b", ks + "c"], w=[ks + "b"])
                    P.op("dve", lambda e, s_=s_: e.tensor_scalar(out=s_[:, 4:5], in0=s_[:, 1:2], scalar1=1.0 / 256, scalar2=RMS_EPS,
                                                                 op0=ALU.mult, op1=ALU.add), r=[ks + "b"], w=[ks + "e"])
                    P.op("act", lambda e, s_=s_: e.activation(out=s_[:, 5:7], in_=s_[:, 3:5], func=AF.Ln),
                         r=[ks + "d", ks + "e"], w=[ks + "f"])
                    P.op("act", lambda e, s_=s_: e.activation(out=s_[:, 3:5], in_=s_[:, 5:7], func=AF.Exp, scale=-0.5), r=[ks + "f"], w=[ks + "d", ks + "e"])
                    P.op("dve", lambda e, pa=pa, s_=s_, qn_=qn_: e.tensor_scalar(out=qn_[:, 0:384], in0=pa[:, 0:384], scalar1=s_[:, 3:4],
                                                                                scalar2=None, op0=ALU.mult), r=[ka, ks + "d"], w=[kq + "a"])
                    P.op("dve", lambda e, pa=pa, s_=s_, qn_=qn_: e.tensor_scalar(out=qn_[:, 384:512], in0=pa[:, 384:512], scalar1=s_[:, 4:5],
                                                                                scalar2=None, op0=ALU.mult), r=[ka, ks + "e"], w=[kq + "b"])
                    P.op("dve", lambda e, pb=pb, s_=s_, qn_=qn_: e.tensor_scalar(out=qn_[:, 512:640], in0=pb[:, 0:128], scalar1=s_[:, 4:5],
                                                                                scalar2=None, op0=ALU.mult), r=[kb_, ks + "e"], w=[kq + "c"])
                    def tail():
                        for k in range(5):
                            P.op("pe", lambda e, k=k, tpb=tpb, qn_=qn_: e.transpose(out=tpb[:, sl(k)], in_=qn_[:, sl(k)], identity=identb[:, :]),
                                 r=[kq + "a", kq + "b", kq + "c", "identb"], w=[kt])
                        P.op("act", lambda e, tpb=tpb: e.activation(out=QANT[:, :, sl(tt)],
                                                                    in_=tpb[:, 0:384].rearrange("p (c t) -> p c t", c=3), func=AF.Copy),
                             r=[kt], w=[("QANT", tt)])
                        P.op("act", lambda e, tpb=tpb: e.activation(out=CKVT[:, :, sl(tt)],
                                                                    in_=tpb[:, 384:640].rearrange("p (c t) -> p c t", c=2), func=AF.Copy),
                             r=[kt], w=[("CKVT", tt)])
                    miq.push(2, tail)
                import os as _os
                _lvl = int(_os.environ.get("MI_LEVEL", "9"))
                for tt in range(NT if _lvl >= 1 else 0):
                    mi_tile(tt)
                    miq.tick()
                miq.flush()

                def mi_kr(tc):
                    pc1, pc2 = PC[0], PC[1]
                    for c in range(8):
                        P.op("pe", lambda e, c=c: e.matmul(pc1[0:96, :], lhsT=WM[:, c, 576:672], rhs=XT[:, c, sl(tc, 512)],
                                                           start=(c == 0), stop=(c == 7)),
                             r=[("XT", 4 * tc + i) for i in range(4)] + ["WM"], w=["pc1"])
                    for c in range(8):
                        P.op("pe", lambda e, c=c: e.matmul(pc2[0:96, :], lhsT=KSW[:, c, 0:96], rhs=XT[:, c, sl(tc, 512)],
                                                           start=(c == 0), stop=(c == 7)),
                             r=[("XT", 4 * tc + i) for i in range(4)] + ["KSW"], w=["pc2"])
                    P.op("dve", lambda e: e.tensor_tensor(out=rt[64:96, 0, :], in0=pc1[64:96, :], in1=COS[64:96, sl(tc, 512)], op=ALU.mult),
                         r=["pc1", "COS"], w=["rt0"])
                    P.op("dve", lambda e: e.tensor_tensor(out=rt[64:96, 1, :], in0=pc2[64:96, :], in1=SIN[64:96, sl(tc, 512)], op=ALU.mult),
                         r=["pc2", "SIN"], w=["rt1"])
                    P.op("dve", lambda e: e.tensor_tensor(out=KRT[64:96, sl(tc, 512)], in0=rt[64:96, 0, :], in1=rt[64:96, 1, :], op=ALU.add),
                         r=["rt0", "rt1"], w=[("KRT", tc)])
                for tc in range(4 if _lvl >= 2 else 0):
                    mi_kr(tc)
                if stop_after == "mi":
                    tap(P, "QANT", QANT[:, :, :], [("QANT", i) for i in range(NT)])
                    tap(P, "CKVT", CKVT[:, :, :], [("CKVT", i) for i in range(NT)])
                    tap(P, "KRT", KRT[64:96, :], [("KRT", i) for i in range(4)])
                P.emit()
            if stop_after == "mi":
                mla.close()
                break
            with ExitStack() as ph:
                psb = lambda name, shape, dt=F32: ph.enter_context(nc.sbuf_tensor(name + sfx[0], list(shape), dt))
                pps = lambda name, shape, dt=F32: ph.enter_context(nc.psum_tensor(name + sfx[0], list(shape), dt))
                WQ = psb("WQ", [128, 3, 768], BF16)
                WQS = psb("WQS", [128, 3, 768], BF16)
                WUK = psb("WUK", [128, 2, 512], BF16)
                WUV = psb("WUV", [128, 2, 512], BF16)
                GQ = psb("GQ", [128, 3])
                GKV = psb("GKV", [128, 2])
                VA = psb("VA", [128, 16, 768], BF16)
                QTH = [psb("QTH%d" % i, [96, T], BF16) for i in range(2)]
                KTH = [psb("KTH%d" % i, [96, T], BF16) for i in range(2)]
                PT = [psb("PT%d" % i, [128, 512], BF16) for i in range(5)]
                OS = [psb("OS%d" % i, [128, 512]) for i in range(2)]
                LINV2 = [psb("LINV%d" % i, [128, 512]) for i in range(2)]
                rt2 = psb("rt2", [96, 2, 512])
                TRI = psb("TRI", [128, 128], BF16)
                ST = [pps("ST%d" % i, [128, 512]) for i in range(3)]
                OB = [pps("OB%d" % i, [128, 512]) for i in range(2)]
                GA = pps("GA", [128, 512])
                GB = pps("GB", [128, 512])
                BCP = pps("BCP", [128, 512])
                wq = w_q_up[l].rearrange("(c p) n -> p c n", p=128)
                P.op("sp", lambda e: e.dma_start(out=TRI[:, :], in_=c_tri), w=["TRI"], chan="TRI")
                P.op("pool", lambda e: e.dma_start(out=WQ[:, :, :], in_=wq), w=["WQ"], chan="WQ")
                P.op("dve", lambda e: e.memset(WQS[:, :, :], 0.0), w=["WQS"])
                wq4 = wq.rearrange("p c (h d) -> p c h d", d=96)
                WQS4 = WQS[:, :, :].rearrange("p c (h d) -> p c h d", d=96)
                for c in range(3):
                    P.op("pool", lambda e, c=c: e.dma_start(out=WQS4[:, c, :, 64:80], in_=wq4[:, c, :, 80:96]), w=["WQS"], chan="WQS")
                    P.op("pool", lambda e, c=c: e.dma_start(out=WQS4[:, c, :, 80:96], in_=wq4[:, c, :, 64:80]), w=["WQS"], chan="WQS")
                P.op("pool", lambda e: e.dma_start(out=WUK[:, :, :], in_=w_uk[l].rearrange("(c p) n -> p c n", p=128)), w=["WUK"], chan="WUK")
                P.op("pool", lambda e: e.dma_start(out=WUV[:, :, :], in_=w_uv[l].rearrange("(c p) n -> p c n", p=128)), w=["WUV"], chan="WUV")
                P.op("sp", lambda e: e.dma_start(out=GQ[:, :], in_=q_norm_g[l].rearrange("(c p) -> p c", p=128),
                                                 allow_slow_non_contiguous=True), w=["GQ"], chan="GQ")
                P.op("sp", lambda e: e.dma_start(out=GKV[:, :], in_=kv_norm_g[l].rearrange("(c p) -> p c", p=128),
                                                 allow_slow_non_contiguous=True), w=["GKV"], chan="GKV")
                for c in range(3):
                    P.op("dve", lambda e, c=c: e.tensor_scalar(out=WQ[:, c, :], in0=WQ[:, c, :], scalar1=GQ[:, c:c + 1], scalar2=None, op0=ALU.mult),
                         r=["GQ"], w=["WQ"])
                    P.op("dve", lambda e, c=c: e.tensor_scalar(out=WQS[:, c, :], in0=WQS[:, c, :], scalar1=GQ[:, c:c + 1], scalar2=None, op0=ALU.mult),
                         r=["GQ"], w=["WQS"])
                for c in range(2):
                    P.op("dve", lambda e, c=c: e.tensor_scalar(out=WUK[:, c, :], in0=WUK[:, c, :], scalar1=GKV[:, c:c + 1], scalar2=None, op0=ALU.mult),
                         r=["GKV"], w=["WUK"])
                    P.op("dve", lambda e, c=c: e.tensor_scalar(out=WUV[:, c, :], in0=WUV[:, c, :], scalar1=GKV[:, c:c + 1], scalar2=None, op0=ALU.mult),
                         r=["GKV"], w=["WUV"])
                P.op("pool", lambda e: e.memset(VA[:, :, :], 0.0), w=["VAc"])
                P.op("pool", lambda e: e.memset(VA[:, :, :].rearrange("p j (a x) -> p (j a) x", x=192)[:, :, 64:65], 1.0), w=["VAc"])

                def v_tile(j):
                    for c in range(2):
                        P.op("pe", lambda e, c=c: e.matmul(GA[:, :], lhsT=CKVT[:, c, sl(j)], rhs=WUV[:, c, :], start=(c == 0), stop=(c == 1)),
                             r=[("CKVT", j), "WUV"], w=["GA"])
                    src = GA[:, :].rearrange("p (a e d) -> p a e d", e=2, d=64)
                    dst = VA[:, j, :].rearrange("p (a x) -> p a x", x=192)
                    P.op("act", lambda e: e.activation(out=dst[:, :, 0:64], in_=src[:, :, 0, :], func=AF.Copy), r=["GA", "VAc"], w=[("VA", j)])
                    P.op("act", lambda e: e.activation(out=dst[:, :, 128:192], in_=src[:, :, 1, :], func=AF.Copy), r=["GA", "VAc"], w=[("VA", j)])
                for j in range(NT):
                    v_tile(j)
                cnt = [0]

                def norm_out(ob, obk, odd, dst, dstk, osb, osk, part, li):
                    lv, lk = LINV2[li], "LINV%d" % li
                    if not odd:
                        if part == 0:
                            P.op("act", lambda e: e.activation(out=osb[0:65, :], in_=ob[0:65, :], func=AF.Copy), r=[obk], w=[osk])
                            P.op("act", lambda e: e.activation(out=lv[64:65, :], in_=osb[64:65, :], func=AF.Ln), r=[osk], w=[lk])
                            P.op("act", lambda e: e.activation(out=lv[64:65, :], in_=lv[64:65, :], func=AF.Exp, scale=-1.0), w=[lk])
                        else:
                            P.op("pe", lambda e: e.matmul(BCP[0:64, :], lhsT=ones_f[64:65, 0:64], rhs=lv[64:65, :], start=True, stop=True),
                                 r=[lk, "ones_f"], w=["BCP"])
                            P.op("dve", lambda e: e.tensor_tensor(out=dst, in0=osb[0:64, :], in1=BCP[0:64, :], op=ALU.mult),
                                 r=[osk, "BCP"], w=[dstk])
                    else:
                        if part == 0:
                            P.op("act", lambda e: e.activation(out=osb[:, :], in_=ob[:, :], func=AF.Copy), r=[obk], w=[osk])
                            P.op("act", lambda e: e.activation(out=lv[0:1, :], in_=osb[0:1, :], func=AF.Ln), r=[osk], w=[lk])
                            P.op("act", lambda e: e.activation(out=lv[0:1, :], in_=lv[0:1, :], func=AF.Exp, scale=-1.0), w=[lk])
                        else:
                            P.op("pe", lambda e: e.matmul(BCP[:, :], lhsT=ones_f[0:1, :], rhs=lv[0:1, :], start=True, stop=True),
                                 r=[lk, "ones_f"], w=["BCP"])
                            P.op("dve", lambda e: e.tensor_tensor(out=dst, in0=osb[64:128, :], in1=BCP[64:128, :], op=ALU.mult),
                                 r=[osk, "BCP"], w=[dstk])

                def attn_chunk(tc, kt_fn, q_fn, v_fn, rk, odd, scale, mask_fn, ob, obk):
                    nj = 4 * tc + 4
                    M = 128 if odd else 65

                    def unit(j):
                        ip = j - 4 * tc
                        t0 = max(0, ip) * 128
                        N = 512 - t0
                        sb_ = cnt[0] % 3
                        pb_ = pcnt[0] % 5
                        cnt[0] += 1
                        pcnt[0] += 1
                        stt, stk, ptt, ptk = ST[sb_], "ST%d" % sb_, PT[pb_], "PT%d" % pb_
                        kap, qap, vap = kt_fn(j), q_fn(t0), v_fn(j)
                        P.op("pe", lambda e: e.matmul(stt[:, 0:N], lhsT=kap, rhs=qap, start=True, stop=True), r=rk, w=[stk])
                        P.op("act", lambda e: e.activation(out=ptt[:, 0:N], in_=stt[:, 0:N], func=AF.Exp, scale=scale), r=[stk], w=[ptk])
                        mask_fn(j, ip, t0, N, ptt, ptk)
                        pend.tick()
                        pend.push(3, lambda: P.op("pe", lambda e: e.matmul(ob[0:M, t0:512], lhsT=vap, rhs=ptt[:, 0:N], start=(j == 0), stop=(j == nj - 1)),
                                                  r=[ptk] + rk, w=[obk]))
                    for j in range(nj):
                        unit(j)

                pend = TickQ()
                ncnt = [0]
                pcnt = [0]

                def mla_gen(h):
                    b = h % 2
                    kth, qth = KTH[b], QTH[b]
                    kk, qk = "KTH%d" % b, "QTH%d" % b

                    def kgen(sc):
                        for c in range(2):
                            P.op("pe", lambda e, c=c: e.matmul(GA[0:64, :], lhsT=WUK[:, c, h * 64:(h + 1) * 64], rhs=CKVT[:, c, sl(sc, 512)],
                                                               start=(c == 0), stop=(c == 1)),
                                 r=[("CKVT", 4 * sc + i) for i in range(4)] + ["WUK"], w=["GA"])
                        P.op("dve", lambda e: e.tensor_copy(out=kth[0:64, sl(sc, 512)], in_=GA[0:64, :]), r=["GA"], w=[kk])
                    for sc in range(4):
                        kgen(sc)
                    P.op("pool", lambda e: e.tensor_copy(out=kth[64:96, :], in_=KRT[64:96, :]), r=[("KRT", i) for i in range(4)], w=[kk])

                    def qgen(tc):
                        for c in range(3):
                            P.op("pe", lambda e, c=c: e.matmul(GA[0:96, :], lhsT=WQ[:, c, h * 96:(h + 1) * 96], rhs=QANT[:, c, sl(tc, 512)],
                                                               start=(c == 0), stop=(c == 2)),
                                 r=[("QANT", 4 * tc + i) for i in range(4)] + ["WQ"], w=["GA"])
                        for c in range(3):
                            P.op("pe", lambda e, c=c: e.matmul(GB[0:96, :], lhsT=WQS[:, c, h * 96:(h + 1) * 96], rhs=QANT[:, c, sl(tc, 512)],
                                                               start=(c == 0), stop=(c == 2)),
                                 r=[("QANT", 4 * tc + i) for i in range(4)] + ["WQS"], w=["GB"])
                        P.op("dve", lambda e: e.tensor_copy(out=qth[0:64, sl(tc, 512)], in_=GA[0:64, :]), r=["GA"], w=[qk])
                        P.op("dve", lambda e: e.tensor_tensor(out=rt2[64:96, 0, :], in0=GA[64:96, :], in1=COS[64:96, sl(tc, 512)], op=ALU.mult),
                             r=["GA", "COS"], w=["rt20"])
                        P.op("dve", lambda e: e.tensor_tensor(out=rt2[64:96, 1, :], in0=GB[64:96, :], in1=SIN[64:96, sl(tc, 512)], op=ALU.mult),
                             r=["GB", "SIN"], w=["rt21"])
                        P.op("dve", lambda e: e.tensor_tensor(out=qth[64:96, sl(tc, 512)], in0=rt2[64:96, 0, :], in1=rt2[64:96, 1, :], op=ALU.add),
                             r=["rt20", "rt21"], w=[qk])
                    for tc in range(4):
                        qgen(tc)

                def mla_head(h):
                    b = h % 2
                    odd = (h % 2 == 1)
                    a = h // 2
                    kth, qth = KTH[b], QTH[b]
                    kk, qk = "KTH%d" % b, "QTH%d" % b

                    def mask_fn(j, ip, t0, N, ptt, ptk):
                        if ip >= 0:
                            P.op("dve", lambda e: e.tensor_tensor(out=ptt[:, 0:128], in0=ptt[:, 0:128], in1=TRI[:, :], op=ALU.mult),
                                 r=["TRI"], w=[ptk])
                    for tc in range(4):
                        ob, obk = OB[tc % 2], "OB%d" % (tc % 2)
                        osb, osk = OS[tc % 2], "OS%d" % (tc % 2)
                        attn_chunk(tc, lambda j: kth[0:96, sl(j)], lambda t0: qth[0:96, tc * 512 + t0:(tc + 1) * 512],
                                   (lambda j: VA[:, j, a * 192 + 64:a * 192 + 192]) if odd else (lambda j: VA[:, j, a * 192:a * 192 + 65]),
                                   [kk, qk] + [("VA", j) for j in range(4 * tc + 4)], odd, MLA_SCALE, mask_fn, ob, obk)
                        dst = MIXT[64:128, a, sl(tc, 512)] if odd else MIXT[0:64, a, sl(tc, 512)]
                        ncnt[0] += 1
                        li = ncnt[0] % 2
                        pend.push(4, lambda ob=ob, obk=obk, dst=dst, tc=tc, osb=osb, osk=osk, li=li: norm_out(ob, obk, odd, dst, ("MIXT", a, tc, odd), osb, osk, 0, li))
                        pend.push(8, lambda ob=ob, obk=obk, dst=dst, tc=tc, osb=osb, osk=osk, li=li: norm_out(ob, obk, odd, dst, ("MIXT", a, tc, odd), osb, osk, 1, li))
                mla_gen(0)
                for h in range(8):
                    if h + 1 < 8:
                        mla_gen(h + 1)
                    mla_head(h)
                pend.flush()
                if stop_after == "ma":
                    tap(P, "MIXA", MIXT[:, 0:4, :], [("MIXT", a, tc, o) for a in range(4) for tc in range(4) for o in (False, True)])
                P.emit()
            mla.close()
            if stop_after == "ma":
                break
            with ExitStack() as ph:
                psb = lambda name, shape, dt=F32: ph.enter_context(nc.sbuf_tensor(name + sfx[0], list(shape), dt))
                pps = lambda name, shape, dt=F32: ph.enter_context(nc.psum_tensor(name + sfx[0], list(shape), dt))
                WD = psb("WD", [128, 8, 936], BF16)
                IK3 = psb("IK3", [128, 8, 96], BF16)
                DKT = psb("DKT", [68, T], BF16)
                ALQ = psb("ALQ", [68, T], BF16)
                IQT = psb("IQT", [96, 3, T], BF16)
                IKT3 = psb("IKT3", [96, T], BF16)
                DVB = psb("DVB", [128, 16, 192], BF16)
                IW = psb("IW", [128, 16, 8])
                AW = psb("AW", [128, 16, 8])
                SG = psb("SG", [128, 16, 8])
                MT2 = [psb("MT%d" % i, [128, 16, 512], BF16) for i in range(2)]
                ACCs = [psb("ACC%d" % i, [128, T]) for i in range(2)]
                MKS = [psb("MK%d" % i, [128, T], BF16) for i in range(4)]
                JKs = [psb("JK0", [128, T], BF16)] * 2
                DQ = [psb("DQ%d" % i, [68, 512], BF16) for i in range(2)]
                PT = [psb("dPT%d" % i, [128, 512], BF16) for i in range(5)]
                OS = [psb("dOS%d" % i, [128, 512]) for i in range(3)]
                LINV2 = [psb("dLINV%d" % i, [128, 512]) for i in range(3)]
                JKA = psb("JKA", [128, T], BF16)
                TRI = psb("dTRI", [128, 128], BF16)
                TRIADD = psb("TRIADD", [128, 128])
                BSs = [psb("BS%d" % i, [128, 8]) for i in range(2)]
                POW = psb("POW", [128, NBIS + 1])
                WKTs = [psb("WKT%d" % i, [128, NBIS + 1]) for i in range(2)]
                WKT2s = [psb("WKT2_%d" % i, [128, NBIS + 1]) for i in range(2)]
                ST = [pps("dST%d" % i, [128, 512]) for i in range(3)]
                OB = [pps("dOB%d" % i, [128, 512]) for i in range(2)]
                GA = pps("dGA", [128, 512])
                BCP = pps("dBCP", [128, 512])
                TPM = pps("TPM", [128, 1024], BF16)
                wl = w_in[l].rearrange("(c p) n -> p c n", p=128)
                for c in range(8):
                    P.op("pool", lambda e, c=c: e.dma_start(out=WD[:, c, :], in_=wl[:, c, 672:1608]), w=["WD"], chan="WD")
                for k in range(3):
                    P.op("pool", lambda e, k=k: e.dma_start(out=IK3[:, :, 32 * k:32 * k + 32], in_=wl[:, :, 1568:1600]), w=["IK3"], chan="IK3")
                P.op("sp", lambda e: e.dma_start(out=TRI[:, :], in_=c_tri), w=["TRI"], chan="dTRI")
                P.op("sp", lambda e: e.dma_start(out=TRIADD[:, :], in_=c_triadd), w=["TRIADD"], chan="TRIADD")
                P.op("sp", lambda e: e.dma_start(out=DKT[64:68, :], in_=c_alk), w=["DKTc"], chan="DKTc")
                P.op("sp", lambda e: e.dma_start(out=ALQ[64:68, :], in_=c_alq), w=["ALQ"], chan="ALQ")
                for kk in range(NBIS + 1):
                    P.op("pool", lambda e, kk=kk: e.memset(POW[:, kk:kk + 1], 2.0 ** -(kk + 2)), w=["POW"])
                P.op("pool", lambda e: e.memset(DVB[:, :, :], 0.0), w=["DVBc"])
                P.op("pool", lambda e: e.memset(DVB[:, :, 64:65], 1.0), w=["DVBc"])

                def d_feat(tc):
                    xk = [("XT", 4 * tc + i) for i in range(4)]

                    def grp(c0, ncol, dst, dk_, wt=WD, wk="WD"):
                        for c in range(8):
                            P.op("pe", lambda e, c=c: e.matmul(GA[0:ncol, :], lhsT=wt[:, c, c0:c0 + ncol], rhs=XT[:, c, sl(tc, 512)],
                                                               start=(c == 0), stop=(c == 7)), r=xk + [wk], w=["GA"])
                        P.op("act", lambda e: e.activation(out=dst, in_=GA[0:ncol, :], func=AF.Copy), r=["GA"], w=[dk_])
                    grp(512, 64, DKT[0:64, sl(tc, 512)], ("DKT", tc))
                    grp(640, 96, IQT[0:96, 0, sl(tc, 512)], ("IQT", tc))
                    grp(736, 96, IQT[0:96, 1, sl(tc, 512)], ("IQT", tc))
                    grp(832, 64, IQT[0:64, 2, sl(tc, 512)], ("IQT", tc))
                    grp(0, 96, IKT3[0:96, sl(tc, 512)], ("IKT", tc), IK3, "IK3")
                for tc in range(4):
                    d_feat(tc)

                def d_tok(tt):
                    for c in range(8):
                        P.op("pe", lambda e, c=c: e.matmul(GA[:, 0:64], lhsT=XT[:, c, sl(tt)], rhs=WD[:, c, 576:640], start=(c == 0), stop=(c == 7)),
                             r=[("XT", tt), "WD"], w=["GA"])
                    for c in range(8):
                        P.op("pe", lambda e, c=c: e.matmul(GA[:, 64:72], lhsT=XT[:, c, sl(tt)], rhs=WD[:, c, 928:936], start=(c == 0), stop=(c == 7)),
                             r=[("XT", tt), "WD"], w=["GA"])
                    P.op("act", lambda e: e.activation(out=DVB[:, tt, 0:64], in_=GA[:, 0:64], func=AF.Copy), r=["GA", "DVBc"], w=[("DVB", tt)])
                    P.op("act", lambda e: e.activation(out=DVB[:, tt, 128:192], in_=GA[:, 0:64], func=AF.Copy), r=["GA", "DVBc"], w=[("DVB", tt)])
                    P.op("act", lambda e: e.activation(out=IW[:, tt, :], in_=GA[:, 64:72], func=AF.Copy), r=["GA"], w=["IW"])
                for tt in range(NT):
                    d_tok(tt)
                P.op("dve", lambda e: e.tensor_scalar(out=SG[:, :, :], in0=IW[:, :, :], scalar1=0.0, scalar2=2.0, op0=ALU.is_ge, op1=ALU.mult),
                     r=["IW"], w=["SG"])
                P.op("dve", lambda e: e.tensor_scalar(out=SG[:, :, :], in0=SG[:, :, :], scalar1=-1.0, scalar2=None, op0=ALU.add), w=["SG"])
                P.op("dve", lambda e: e.scalar_tensor_tensor(out=AW[:, :, :], in0=IW[:, :, :], scalar=1.0 / 16, in1=SG[:, :, :], op0=ALU.mult, op1=ALU.mult),
                     r=["IW", "SG"], w=["AW"])
                cnt = [0]
                ncnt = [0]
                pcnt = [0]
                pend = TickQ()

                def idx_pair(tiles, tc):
                    info = []
                    for slot, i in enumerate(tiles):
                        il = i - 4 * tc
                        nk = (i + 1) * 128
                        nsc = (nk + 511) // 512
                        ACC, BS, WKT, WKT2, JK = ACCs[slot], BSs[slot], WKTs[slot], WKT2s[slot], JKs[slot]
                        bk = "bis%d" % slot

                        def one(h, sc, i=i, nk=nk, ACC=ACC, slot=slot):
                            k, pbase = h // 3, 32 * (h % 3)
                            wdt = min(512, nk - sc * 512)
                            sb_ = cnt[0] % 3
                            cnt[0] += 1
                            stt, stk = ST[sb_], "ST%d" % sb_
                            P.op("pe", lambda e: e.matmul(stt[:, 0:wdt], lhsT=IQT[pbase:pbase + 32, k, sl(i)], rhs=IKT3[pbase:pbase + 32, sc * 512:sc * 512 + wdt],
                                                          start=True, stop=True),
                                 r=[("IQT", tc)] + [("IKT", q) for q in range(tc + 1)], w=[stk])
                            P.op("act", lambda e: e.activation(out=stt[:, 0:wdt], in_=stt[:, 0:wdt], func=AF.Relu, scale=AW[:, i, h:h + 1]),
                                 r=[stk, "AW"], w=[stk])
                            if h == 0:
                                P.op("dve", lambda e: e.tensor_scalar(out=ACC[:, sc * 512:sc * 512 + wdt], in0=stt[:, 0:wdt], scalar1=SG[:, i, 0:1], scalar2=None,
                                                                      op0=ALU.mult), r=[stk, "SG"], w=[("ACC", slot, sc)])
                            else:
                                P.op("dve", lambda e: e.scalar_tensor_tensor(out=ACC[:, sc * 512:sc * 512 + wdt], in0=stt[:, 0:wdt], scalar=SG[:, i, h:h + 1],
                                                                             in1=ACC[:, sc * 512:sc * 512 + wdt], op0=ALU.mult, op1=ALU.add),
                                     r=[stk, "SG"], w=[("ACC", slot, sc)])
                        for h in range(8):
                            for sc in range(nsc):
                                one(h, sc)
                                yield
                        acck = [("ACC", slot, sc) for sc in range(nsc)]

                        def prep(i=i, nk=nk, ACC=ACC, BS=BS, WKT=WKT, WKT2=WKT2, bk=bk, acck=acck, slot=slot):
                            P.op("dve", lambda e: e.tensor_reduce(out=BS[:, 0:1], in_=ACC[:, 0:nk], axis=AX.X, op=ALU.max, apply_absolute_value=True),
                                 r=acck, w=[bk])
                            P.op("dve", lambda e: e.tensor_scalar(out=BS[:, 1:2], in0=BS[:, 0:1], scalar1=2.0, scalar2=2.0, op0=ALU.mult, op1=ALU.add), w=[bk])
                            P.op("dve", lambda e: e.tensor_scalar(out=WKT[:, :], in0=POW[:, :], scalar1=BS[:, 1:2], scalar2=None, op0=ALU.mult), r=["POW"], w=[bk])
                            P.op("dve", lambda e: e.tensor_scalar(out=WKT2[:, :], in0=POW[:, :], scalar1=BS[:, 1:2], scalar2=2.0, op0=ALU.mult, op1=ALU.mult),
                                 r=["POW"], w=[bk])
                            P.op("dve", lambda e: e.memset(BS[:, 2:3], 0.0), w=[bk])
                            P.op("pool", lambda e: e.tensor_tensor(out=ACC[:, sl(i)], in0=ACC[:, sl(i)], in1=TRIADD[:, :], op=ALU.add),
                                 r=["TRIADD", bk], w=[("ACC", slot, i // 4)])
                        prep()
                        info.append((i, il, nk, acck, ACC, BS, WKT, WKT2, JK, bk, slot))

                    def bis(kk, t):
                        i, il, nk, acck, ACC, BS, WKT, WKT2, JK, bk, slot = t
                        if slot == 1:
                            P.op("act", lambda e: e.activation(out=JKA[:, 0:nk], in_=ACC[:, 0:nk], func=AF.Sign, bias=BS[:, 2:3], scale=1.0,
                                                               accum_out=BS[:, 5:6]), r=acck + [bk], w=[bk + "c", "JKa"])
                            P.op("dve", lambda e: e.tensor_scalar(out=BS[:, 6:7], in0=BS[:, 5:6], scalar1=float(512 - nk), scalar2=WKT2[:, kk:kk + 1],
                                                                  op0=ALU.is_lt, op1=ALU.mult), r=[bk + "c"], w=[bk])
                            P.op("dve", lambda e: e.scalar_tensor_tensor(out=BS[:, 2:3], in0=BS[:, 6:7], scalar=WKT[:, kk:kk + 1], in1=BS[:, 2:3],
                                                                         op0=ALU.subtract, op1=ALU.add), w=[bk])
                            return
                        P.op("dve", lambda e: e.tensor_scalar(out=JK[:, 0:nk], in0=ACC[:, 0:nk], scalar1=BS[:, 2:3], scalar2=None, op0=ALU.is_ge,
                                                              op1=ALU.add, accum_out=BS[:, 5:6]), r=acck, w=[bk, "JK%d" % slot])
                        P.op("dve", lambda e: e.tensor_scalar(out=BS[:, 6:7], in0=BS[:, 5:6], scalar1=256.0, scalar2=WKT2[:, kk:kk + 1],
                                                              op0=ALU.is_ge, op1=ALU.mult), w=[bk])
                        P.op("dve", lambda e: e.scalar_tensor_tensor(out=BS[:, 2:3], in0=BS[:, 6:7], scalar=WKT[:, kk:kk + 1], in1=BS[:, 2:3],
                                                                     op0=ALU.subtract, op1=ALU.add), w=[bk])
                    for kk in range(NBIS):
                        for t in info:
                            bis(kk, t)
                        yield

                    def fin(t):
                        i, il, nk, acck, ACC, BS, WKT, WKT2, JK, bk, slot = t
                        if slot == 1:
                            P.op("dve", lambda e: e.tensor_scalar(out=BS[:, 2:3], in0=BS[:, 2:3], scalar1=-1.0, scalar2=WKT2[:, NBIS:NBIS + 1],
                                                                  op0=ALU.mult, op1=ALU.subtract), w=[bk])
                        else:
                            P.op("dve", lambda e: e.tensor_tensor(out=BS[:, 2:3], in0=BS[:, 2:3], in1=WKT2[:, NBIS:NBIS + 1], op=ALU.subtract), w=[bk])
                        MK = MKS[il]
                        P.op("dve", lambda e: e.tensor_scalar(out=MK[:, 0:nk], in0=ACC[:, 0:nk], scalar1=BS[:, 2:3], scalar2=None, op0=ALU.is_ge),
                             r=acck + [bk], w=[("MK", il)])
                    for t in info:
                        fin(t)
                    yield

                def idx_steps(tiles):
                    return sum(8 * (((i + 1) * 128 + 511) // 512) for i in tiles) + NBIS + 1

                def idx_transposes(i, tc):
                    il = i - 4 * tc
                    MK = MKS[il]
                    MT = MT2[tc % 2]

                    def tgrp(j0, n):
                        for jj in range(n):
                            P.op("pe", lambda e, jj=jj: e.transpose(out=TPM[:, sl(jj)], in_=MK[:, sl(j0 + jj)], identity=identb[:, :]),
                                 r=[("MK", il), "identb"], w=["TPM"])
                        P.op("act", lambda e: e.activation(out=MT[:, j0:j0 + n, sl(il)], in_=TPM[:, 0:n * 128].rearrange("p (j t) -> p j t", t=128),
                                                           func=AF.Copy), r=["TPM"], w=[("MT", tc % 2, il)])
                    for j0 in range(0, i + 1, 8):
                        tgrp(j0, min(8, i + 1 - j0))

                def idx_special(i):
                    MT = MT2[0]
                    if i == 0:
                        P.op("pool", lambda e: e.tensor_copy(out=MT[:, 0, 0:128], in_=TRI[:, :]), r=["TRI"], w=[("MT", 0, 0)])
                    else:
                        P.op("pool", lambda e: e.memset(MT[:, 0, 128:256], 1.0), w=[("MT", 0, 1)])
                        P.op("pool", lambda e: e.tensor_copy(out=MT[:, 1, 128:256], in_=TRI[:, :]), r=["TRI"], w=[("MT", 0, 1)])

                def dsa_chunk(tc):
                    MT = MT2[tc % 2]
                    mtk = [("MT", tc % 2, q) for q in range(4)]
                    nxt = [i for i in range(4 * tc + 4, 4 * tc + 8)] if tc < 3 else []
                    import itertools, math
                    if nxt:
                        gen = itertools.chain(idx_pair(nxt[0:2], tc + 1), idx_pair(nxt[2:4], tc + 1))
                        rate = int(math.ceil((idx_steps(nxt[0:2]) + idx_steps(nxt[2:4])) / float(8 * (4 * tc + 4) - 8)))
                    else:
                        gen, rate = iter(()), 0

                    def dqgen(h):
                        b = h % 2
                        dq, dqk = DQ[b], "DQ%d" % b
                        for c in range(8):
                            P.op("pe", lambda e, c=c: e.matmul(GA[0:64, :], lhsT=WD[:, c, h * 64:(h + 1) * 64], rhs=XT[:, c, sl(tc, 512)],
                                                               start=(c == 0), stop=(c == 7)),
                                 r=[("XT", 4 * tc + q) for q in range(4)] + ["WD"], w=["GA"])
                        P.op("act", lambda e: e.mul(out=dq[0:64, :], in_=GA[0:64, :], mul=0.125 * 2.0 ** (h + 1)), r=["GA"], w=[dqk])
                        P.op("pool", lambda e: e.tensor_copy(out=dq[64:68, :], in_=ALQ[64:68, sl(tc, 512)]), r=["ALQ"], w=[dqk])

                    def head(h):
                        b = h % 2
                        odd = (h % 2 == 1)
                        dq, dqk = DQ[b], "DQ%d" % b
                        ucount = [0]

                        def mask_fn(j, ip, t0, N, ptt, ptk):
                            P.op("pool", lambda e: e.tensor_tensor(out=ptt[:, 0:N], in0=ptt[:, 0:N], in1=MT[:, j, t0:512], op=ALU.mult),
                                 r=mtk, w=[ptk])
                            for _ in range(rate):
                                next(gen, None)
                        ob, obk = OB[h % 2], "OB%d" % (h % 2)
                        osb, osk = OS[ncnt[0] % 3], "OS%d" % (ncnt[0] % 3)
                        attn_chunk2(tc, lambda j: DKT[0:68, sl(j)], lambda t0: dq[0:68, t0:512],
                                    (lambda j: DVB[:, j, 64:192]) if odd else (lambda j: DVB[:, j, 0:65]),
                                    [dqk, "DKTc"] + [("DKT", q) for q in range(tc + 1)] + [("DVB", j) for j in range(4 * tc + 4)],
                                    odd, 2.0 ** -(h + 1), mask_fn, ob, obk)
                        a = 4 + h // 2
                        dst = MIXT[64:128, a, sl(tc, 512)] if odd else MIXT[0:64, a, sl(tc, 512)]
                        li = ncnt[0] % 3
                        ncnt[0] += 1
                        pend.push(4, lambda: norm_out2(ob, obk, odd, dst, ("MIXT", a, tc, odd), osb, osk, 0, li))
                        pend.push(8, lambda: norm_out2(ob, obk, odd, dst, ("MIXT", a, tc, odd), osb, osk, 1, li))
                    dqgen(0)
                    for h in range(8):
                        if h + 1 < 8:
                            dqgen(h + 1)
                        head(h)
                    for _ in gen:
                        pass
                    pend.flush()
                    for i in nxt:
                        idx_transposes(i, tc + 1)

                def norm_out2(ob, obk, odd, dst, dstk, osb, osk, part, li):
                    lv, lk = LINV2[li], "LINV%d" % li
                    if not odd:
                        if part == 0:
                            P.op("act", lambda e: e.activation(out=osb[0:65, :], in_=ob[0:65, :], func=AF.Copy), r=[obk], w=[osk])
                            P.op("act", lambda e: e.activation(out=lv[64:65, :], in_=osb[64:65, :], func=AF.Ln), r=[osk], w=[lk])
                            P.op("act", lambda e: e.activation(out=lv[64:65, :], in_=lv[64:65, :], func=AF.Exp, scale=-1.0), w=[lk])
                        else:
                            P.op("pe", lambda e: e.matmul(BCP[0:64, :], lhsT=ones_f[64:65, 0:64], rhs=lv[64:65, :], start=True, stop=True),
                                 r=[lk, "ones_f"], w=["BCP"])
                            P.op("dve", lambda e: e.tensor_tensor(out=dst, in0=osb[0:64, :], in1=BCP[0:64, :], op=ALU.mult),
                                 r=[osk, "BCP"], w=[dstk])
                    else:
                        if part == 0:
                            P.op("act", lambda e: e.activation(out=osb[:, :], in_=ob[:, :], func=AF.Copy), r=[obk], w=[osk])
                            P.op("act", lambda e: e.activation(out=lv[0:1, :], in_=osb[0:1, :], func=AF.Ln), r=[osk], w=[lk])
                            P.op("act", lambda e: e.activation(out=lv[0:1, :], in_=lv[0:1, :], func=AF.Exp, scale=-1.0), w=[lk])
                        else:
                            P.op("pe", lambda e: e.matmul(BCP[:, :], lhsT=ones_f[0:1, :], rhs=lv[0:1, :], start=True, stop=True),
                                 r=[lk, "ones_f"], w=["BCP"])
                            P.op("dve", lambda e: e.tensor_tensor(out=dst, in0=osb[64:128, :], in1=BCP[64:128, :], op=ALU.mult),
                                 r=[osk, "BCP"], w=[dstk])

                def attn_chunk2(tc, kt_fn, q_fn, v_fn, rk, odd, scale, mask_fn, ob, obk):
                    nj = 4 * tc + 4
                    M = 128 if odd else 65

                    def unit(j):
                        ip = j - 4 * tc
                        t0 = max(0, ip) * 128
                        N = 512 - t0
                        sb_ = cnt[0] % 3
                        pb_ = pcnt[0] % 5
                        cnt[0] += 1
                        pcnt[0] += 1
                        stt, stk, ptt, ptk = ST[sb_], "ST%d" % sb_, PT[pb_], "PT%d" % pb_
                        kap, qap, vap = kt_fn(j), q_fn(t0), v_fn(j)
                        P.op("pe", lambda e: e.matmul(stt[:, 0:N], lhsT=kap, rhs=qap, start=True, stop=True), r=rk, w=[stk])
                        P.op("act", lambda e: e.activation(out=ptt[:, 0:N], in_=stt[:, 0:N], func=AF.Exp, scale=scale), r=[stk], w=[ptk])
                        mask_fn(j, ip, t0, N, ptt, ptk)
                        pend.tick()
                        pend.push(3, lambda: P.op("pe", lambda e: e.matmul(ob[0:M, t0:512], lhsT=vap, rhs=ptt[:, 0:N], start=(j == 0), stop=(j == nj - 1)),
                                                  r=[ptk] + rk, w=[obk]))
                    for j in range(nj):
                        unit(j)
                idx_special(0)
                idx_special(1)
                for _ in idx_pair([2, 3], 0):
                    pass
                idx_transposes(2, 0)
                idx_transposes(3, 0)
                for tc in range(4):
                    dsa_chunk(tc)
                if stop_after == "da":
                    tap(P, "MIXB", MIXT[:, 4:8, :], [("MIXT", a, tc, o) for a in range(4, 8) for tc in range(4) for o in (False, True)])
                P.emit()
            if stop_after == "da":
                break
            with ExitStack() as ph:
                psb = lambda name, shape, dt=F32: ph.enter_context(nc.sbuf_tensor(name + sfx[0], list(shape), dt))
                pps = lambda name, shape, dt=F32: ph.enter_context(nc.psum_tensor(name + sfx[0], list(shape), dt))
                WO = psb("WO", [128, 8, D], BF16)
                LG_ = psb("LNG", [128, D])
                LB_ = psb("LNB", [128, D])
                RW = psb("RW", [128, 8, NE])
                RB = psb("RB", [128, NE])
                xin = [psb("w_xin%d" % i, [128, D]) for i in range(2)]
                ys = [psb("w_ys%d" % i, [128, D]) for i in range(2)]
                xo = [psb("w_xo%d" % i, [128, D]) for i in range(2)]
                XRs = [psb("XR%d" % i, [128, 8, 128]) for i in range(2)]
                wq_ = TickQ()
                st6 = psb("w_st6", [128, 12])
                mv = psb("w_mv", [128, 8])
                rs = psb("w_rs", [128, 12, NE])
                YP = [pps("YP%d" % i, [128, D]) for i in range(2)]
                tp0 = pps("w_tp0", [128, 512])
                tp1 = pps("w_tp1", [128, 512])
                LGP = pps("LGP", [128, 512])
                wo = w_o[l].rearrange("(c p) n -> p c n", p=128)
                for c in range(8):
                    P.op("pool", lambda e, c=c: e.dma_start(out=WO[:, c, :], in_=wo[:, c, :]), w=["WO"], chan="WO")
                P.op("sp", lambda e: e.dma_start(out=LG_[:, :], in_=ln1_g[l:l + 1, :].to_broadcast([128, D])), w=["lng"], chan="lng")
                P.op("sp", lambda e: e.dma_start(out=LB_[:, :], in_=ln1_b[l:l + 1, :].to_broadcast([128, D])), w=["lnb"], chan="lnb")
                P.op("sp", lambda e: e.dma_start(out=RW[:, :, :], in_=router_w.rearrange("(c p) n -> p c n", p=128)), w=["RW"], chan="RW")
                P.op("sp", lambda e: e.dma_start(out=RB[:, :], in_=router_bias[0:1, :].to_broadcast([128, NE])), w=["RB"], chan="RB")

                import os as _os
                _wl = int(_os.environ.get("W_LEVEL", "9"))

                def w_tile(tt):
                    b = tt % 2
                    xi, xik, y, yk, xo_, xok, yp, ypk = xin[b], "xin%d" % b, ys[b], "ys%d" % b, xo[b], "xo%d" % b, YP[b], "YP%d" % b
                    if _wl < 1:
                        return
                    P.op("sp", lambda e: e.dma_start(out=xi[:, :], in_=xsrc[sl(tt), :]), w=[xik], chan=xik)
                    for n in range(2):
                        for c in range(8):
                            P.op("pe", lambda e, n=n, c=c: e.matmul(yp[:, sl(n, 512)], lhsT=MIXT[:, c, sl(tt)], rhs=WO[:, c, sl(n, 512)],
                                                                    start=(c == 0), stop=(c == 7)),
                                 r=[("MIXT", a, tt // 4, o) for a in range(8) for o in (False, True)] + ["WO"], w=[ypk])
                    P.op("dve", lambda e: e.scalar_tensor_tensor(out=y[:, :], in0=xi[:, :], scalar=ALPHA, in1=yp[:, :], op0=ALU.mult, op1=ALU.add),
                         r=[xik, ypk], w=[yk])
                    emit_layernorm(P, "w", y, xo_, LG_, LB_, st6, mv, yk, xok)
                    if _wl < 2:
                        return
                    P.op("pool", lambda e: e.dma_start(out=xs1[sl(tt), :], in_=xo_[:, :]), r=[xok], w=["xs1_%d" % tt], chan="xs1w%d" % b)
                    if _wl < 3:
                        return

                    XR = XRs[b]

                    def extra(half, tp, kb):
                        P.op("act", lambda e: e.activation(out=XR[:, half * 4:half * 4 + 4, :], in_=tp[:, :].rearrange("p (c t) -> p c t", c=4), func=AF.Copy),
                             r=[kb], w=["XR%d_%d" % (b, half)])
                    wq_.push(2, lambda: emit_xt_update(P, xo_, xok, XT, tt, identf, tp0, tp1, extra))
                    wq_.push(3, lambda: w_router(tt, b))

                def w_router(tt, b):
                    XR = XRs[b]
                    for c in range(8):
                        P.op("pe", lambda e, c=c: e.matmul(LGP[:, 0:NE], lhsT=XR[:, c, :], rhs=RW[:, c, :], start=(c == 0), stop=(c == 7)),
                             r=["XR%d_0" % b, "XR%d_1" % b, "RW"], w=["LGP"])
                    A, Bi = rs[:, 0, :], rs[:, 1, :]
                    k = "rs"
                    P.op("act", lambda e: e.activation(out=A, in_=LGP[:, 0:NE], func=AF.Exp, scale=-1.0), r=["LGP"], w=[k])
                    P.op("dve", lambda e: e.tensor_scalar(out=A, in0=A, scalar1=1.0, scalar2=None, op0=ALU.add), w=[k])
                    P.op("dve", lambda e: e.reciprocal(out=A, in_=A), w=[k])
                    P.op("dve", lambda e: e.tensor_tensor(out=Bi, in0=A, in1=RB[:, :], op=ALU.add), r=["RB"], w=[k])
                    B4 = rs[:, 1, :].rearrange("p (g x) -> p g x", x=4)
                    m1, n1, m2, n2 = rs[:, 2, 0:4], rs[:, 2, 4:8], rs[:, 2, 8:12], rs[:, 2, 12:16]
                    P.op("dve", lambda e: e.tensor_tensor(out=m1, in0=B4[:, :, 0], in1=B4[:, :, 1], op=ALU.max), w=[k])
                    P.op("dve", lambda e: e.tensor_tensor(out=n1, in0=B4[:, :, 0], in1=B4[:, :, 1], op=ALU.min), w=[k])
                    P.op("dve", lambda e: e.tensor_tensor(out=m2, in0=B4[:, :, 2], in1=B4[:, :, 3], op=ALU.max), w=[k])
                    P.op("dve", lambda e: e.tensor_tensor(out=n2, in0=B4[:, :, 2], in1=B4[:, :, 3], op=ALU.min), w=[k])
                    t1, t2, t3, gs = rs[:, 3, 0:4], rs[:, 3, 4:8], rs[:, 3, 8:12], rs[:, 3, 12:16]
                    P.op("dve", lambda e: e.tensor_tensor(out=t1, in0=m1, in1=m2, op=ALU.max), w=[k])
                    P.op("dve", lambda e: e.tensor_tensor(out=t2, in0=m1, in1=m2, op=ALU.min), w=[k])
                    P.op("dve", lambda e: e.tensor_tensor(out=t3, in0=n1, in1=n2, op=ALU.max), w=[k])
                    P.op("dve", lambda e: e.tensor_tensor(out=t2, in0=t2, in1=t3, op=ALU.max), w=[k])
                    P.op("dve", lambda e: e.tensor_tensor(out=gs, in0=t1, in1=t2, op=ALU.add), w=[k])
                    gm = rs[:, 4, 0:1]
                    P.op("dve", lambda e: e.tensor_reduce(out=gm, in_=gs, axis=AX.X, op=ALU.max), w=[k])
                    gmask = rs[:, 4, 4:8]
                    P.op("dve", lambda e: e.tensor_scalar(out=gmask, in0=gs, scalar1=gm, scalar2=None, op0=ALU.is_ge), w=[k])
                    IG = rs[:, 5, :]
                    IG4 = rs[:, 5, :].rearrange("p (g x) -> p g x", x=4)
                    for xx in range(4):
                        P.op("dve", lambda e, xx=xx: e.tensor_copy(out=IG4[:, :, xx], in_=gmask), w=[k])
                    MB = rs[:, 6, :]
                    P.op("dve", lambda e: e.scalar_tensor_tensor(out=MB, in0=Bi, scalar=4.0, in1=IG, op0=ALU.add, op1=ALU.mult), w=[k])
                    tm = rs[:, 4, 1:2]
                    S1, S2 = rs[:, 7, :], rs[:, 8, :]
                    P.op("dve", lambda e: e.tensor_reduce(out=tm, in_=MB, axis=AX.X, op=ALU.max), w=[k])
                    P.op("dve", lambda e: e.tensor_scalar(out=S1, in0=MB, scalar1=tm, scalar2=None, op0=ALU.is_ge), w=[k])
                    P.op("dve", lambda e: e.scalar_tensor_tensor(out=MB, in0=S1, scalar=-8.0, in1=MB, op0=ALU.mult, op1=ALU.add), w=[k])
                    P.op("dve", lambda e: e.tensor_reduce(out=tm, in_=MB, axis=AX.X, op=ALU.max), w=[k])
                    P.op("dve", lambda e: e.tensor_scalar(out=S2, in0=MB, scalar1=tm, scalar2=None, op0=ALU.is_ge), w=[k])
                    P.op("dve", lambda e: e.tensor_tensor(out=S1, in0=S1, in1=S2, op=ALU.add), w=[k])
                    WT = rs[:, 9, :]
                    P.op("dve", lambda e: e.tensor_tensor(out=WT, in0=A, in1=S1, op=ALU.mult), w=[k])
                    ws = rs[:, 4, 2:3]
                    P.op("dve", lambda e: e.tensor_reduce(out=ws, in_=WT, axis=AX.X, op=ALU.add), w=[k])
                    P.op("dve", lambda e: e.reciprocal(out=ws, in_=ws), w=[k])
                    P.op("dve", lambda e: e.tensor_scalar(out=GATES[:, tt, :], in0=WT, scalar1=ws, scalar2=None, op0=ALU.mult), w=[k, ("GATES", tt)])
                for tt in range(NT):
                    w_tile(tt)
                    wq_.tick()
                wq_.flush()
                if stop_after == "w":
                    tap(P, "GATES", GATES[:, :, :], [("GATES", i) for i in range(NT)])
                P.emit()
            if stop_after == "w":
                break
            with ExitStack() as ph:
                psb = lambda name, shape, dt=F32: ph.enter_context(nc.sbuf_tensor(name + sfx[0], list(shape), dt))
                pps = lambda name, shape, dt=F32: ph.enter_context(nc.psum_tensor(name + sfx[0], list(shape), dt))
                MACC = psb("MACC", [128, NT, D])
                WG = [psb("WG%d" % i, [128, 8, DFF], BF16) for i in range(2)]
                WU = [psb("WU%d" % i, [128, 8, DFF], BF16) for i in range(2)]
                WDN = [psb("WDN%d" % i, [128, 4, D], BF16) for i in range(2)]
                AT = [psb("AT%d" % i, [128, 4, 512], BF16) for i in range(2)]
                SGT = [psb("SGT%d" % i, [128, 512]) for i in range(2)]
                LG_ = psb("LNG2", [128, D])
                LB_ = psb("LNB2", [128, D])
                xin = [psb("e_xin%d" % i, [128, D]) for i in range(2)]
                xo = xin
                st6 = psb("e_st6", [128, 12])
                mv = psb("e_mv", [128, 8])
                HG = [pps("HG%d" % i, [128, 512]) for i in range(2)]
                HU = [pps("HU%d" % i, [128, 512]) for i in range(2)]
                YO = [pps("YO%d" % i, [128, 512]) for i in range(2)]
                tp0 = pps("e_tp0", [128, 512])
                tp1 = pps("e_tp1", [128, 512])
                P.op("sp", lambda e: e.dma_start(out=LG_[:, :], in_=ln2_g[l:l + 1, :].to_broadcast([128, D])), w=["lng"], chan="lng2")
                P.op("sp", lambda e: e.dma_start(out=LB_[:, :], in_=ln2_b[l:l + 1, :].to_broadcast([128, D])), w=["lnb"], chan="lnb2")
                cnt = [0]

                eq = TickQ()

                def expert(ex):
                    b = ex % 2
                    wg, wu, wd = WG[b], WU[b], WDN[b]
                    kg, ku, kd = "WG%d" % b, "WU%d" % b, "WDN%d" % b
                    import os as _os
                    _md = _os.environ.get("MOE_DBG", "")
                    if "hwdge" in _md:
                        q_ = "pool" if "swq" in _md else "sp"
                        P.op(q_, lambda e: e.dma_start(out=wg[:, :, :], in_=w_gate[l, ex].bitcast(BF16).rearrange("(c p) n -> p c n", p=128)[:, :, 0:512]), w=[kg], chan=kg)
                        P.op(q_, lambda e: e.dma_start(out=wu[:, :, :], in_=w_up[l, ex].bitcast(BF16).rearrange("(c p) n -> p c n", p=128)[:, :, 0:512]), w=[ku], chan=ku)
                        P.op(q_, lambda e: e.dma_start(out=wd[:, :, :], in_=w_down[l, ex].bitcast(BF16).rearrange("(c p) n -> p c n", p=128)[:, :, 0:1024]), w=[kd], chan=kd)
                    elif "nodma" not in _md or ex < 2:
                        P.op("pool", lambda e: e.dma_start(out=wg[:, :, :], in_=w_gate[l, ex].rearrange("(c p) n -> p c n", p=128)), w=[kg], chan=kg)
                        P.op("pool", lambda e: e.dma_start(out=wu[:, :, :], in_=w_up[l, ex].rearrange("(c p) n -> p c n", p=128)), w=[ku], chan=ku)
                        P.op("pool", lambda e: e.dma_start(out=wd[:, :, :], in_=w_down[l, ex].rearrange("(c p) n -> p c n", p=128)), w=[kd], chan=kd)

                    def chunk(tc):
                        ab = cnt[0] % 2
                        cnt[0] += 1
                        at, atk = AT[ab], "AT%d" % ab
                        xk = [("XT", 4 * tc + q) for q in range(4)]

                        def fchunk(fc):
                            hb = fc % 2
                            hg, hu, sg = HG[hb], HU[hb], SGT[hb]
                            for c in range(8):
                                P.op("pe", lambda e, c=c: e.matmul(hg[:, :], lhsT=wg[:, c, sl(fc)], rhs=XT[:, c, sl(tc, 512)], start=(c == 0), stop=(c == 7)),
                                     r=xk + [kg], w=["HG%d" % hb])
                            for c in range(8):
                                P.op("pe", lambda e, c=c: e.matmul(hu[:, :], lhsT=wu[:, c, sl(fc)], rhs=XT[:, c, sl(tc, 512)], start=(c == 0), stop=(c == 7)),
                                     r=xk + [ku], w=["HU%d" % hb])
                            if "noact" in _md:
                                return
                            P.op("act", lambda e: e.activation(out=sg[:, :], in_=hg[:, :], func=AF.Silu), r=["HG%d" % hb], w=["SGT%d" % hb])
                            P.op("dve", lambda e: e.tensor_tensor(out=at[:, fc, :], in0=sg[:, :], in1=hu[:, :], op=ALU.mult),
                                 r=["SGT%d" % hb, "HU%d" % hb], w=[atk])
                        for fc in range(4):
                            fchunk(fc)

                        def down(tl, n):
                            tt = 4 * tc + tl
                            yb = (tl * 2 + n) % 2
                            yo = YO[yb]
                            for fc in range(4):
                                P.op("pe", lambda e, fc=fc: e.matmul(yo[:, :], lhsT=at[:, fc, sl(tl)], rhs=wd[:, fc, sl(n, 512)], start=(fc == 0), stop=(fc == 3)),
                                     r=[atk, kd], w=["YO%d" % yb])
                            if "noevac" in _md:
                                return
                            if ex == 0:
                                P.op("dve", lambda e: e.tensor_scalar(out=MACC[:, tt, sl(n, 512)], in0=yo[:, :], scalar1=GATES[:, tt, ex:ex + 1], scalar2=None,
                                                                      op0=ALU.mult), r=["YO%d" % yb, ("GATES", tt)], w=[("MACC", tt, n)])
                            else:
                                P.op("dve", lambda e: e.scalar_tensor_tensor(out=MACC[:, tt, sl(n, 512)], in0=yo[:, :], scalar=GATES[:, tt, ex:ex + 1],
                                                                             in1=MACC[:, tt, sl(n, 512)], op0=ALU.mult, op1=ALU.add),
                                     r=["YO%d" % yb, ("GATES", tt)], w=[("MACC", tt, n)])
                        def downs():
                            for tl in range(4):
                                for n in range(2):
                                    down(tl, n)
                            if ex == NE - 1:
                                for tl in range(4):
                                    e_tile(4 * tc + tl)
                        eq.push(2, downs)
                    return chunk

                def e_tile(tt):
                    b = tt % 2
                    xi, xik, xo_, xok = xin[b], "xin%d" % b, xo[b], "xin%d" % b
                    P.op("sp", lambda e: e.dma_start(out=xi[:, :], in_=xs1[sl(tt), :]), w=[xik], chan="e" + xik)
                    P.op("dve", lambda e: e.scalar_tensor_tensor(out=xi[:, :], in0=xi[:, :], scalar=ALPHA, in1=MACC[:, tt, :], op0=ALU.mult, op1=ALU.add),
                         r=[("MACC", tt, 0), ("MACC", tt, 1)], w=[xik])
                    emit_layernorm(P, "e", xi, xo_, LG_, LB_, st6, mv, xik, xok)
                    P.op("pool", lambda e: e.dma_start(out=xdst2[sl(tt), :], in_=xo_[:, :]), r=[xok], w=["xd2_%d" % tt], chan="xd2w%d" % b)
                    if l < n_layers - 1:
                        emit_xt_update(P, xo_, xok, XT, tt, identf, tp0, tp1)
                for ex in range(NE - 2):
                    ch = expert(ex)
                    for tc in range(4):
                        ch(tc)
                        eq.tick()
                cha = expert(NE - 2)
                chb = None
                for tc in range(4):
                    cha(tc)
                    eq.tick()
                    if chb is None:
                        chb = expert(NE - 1)
                    chb(tc)
                    eq.tick()
                eq.flush()
                P.emit()
    return nc, dt_in, tap_aps


def host_constants():
    c = {}
    c["c_identf"] = np.eye(128, dtype=np.float32)
    c["c_identb"] = np.eye(128, dtype=np.float32).astype(ml_dtypes.bfloat16)
    half = 16
    inv = (10000.0 ** (-np.arange(half, dtype=np.float32) / half)).astype(np.float32)
    ang = (np.arange(T, dtype=np.float32)[None, :] * inv[:, None]).astype(np.float32)
    cs = np.cos(ang).astype(np.float32)
    sn = np.sin(ang).astype(np.float32)
    c["c_cos"] = np.concatenate([cs, cs], axis=0)
    c["c_sin"] = np.concatenate([-sn, sn], axis=0)
    s_idx = np.arange(128)
    c["c_tri"] = (s_idx[:, None] <= s_idx[None, :]).astype(np.float32).astype(ml_dtypes.bfloat16)
    c["c_triadd"] = np.where(s_idx[None, :] <= s_idx[:, None], 0.0, -BIG).astype(np.float32)
    t = np.arange(T)
    alq = np.stack([-(64.0 * (t // 64)), -(t % 64).astype(np.float64), np.ones(T), np.ones(T)]).astype(np.float32)
    alk = np.stack([np.ones(T), np.ones(T), 64.0 * (t // 64), (t % 64).astype(np.float64)]).astype(np.float32)
    c["c_alq"] = alq.astype(ml_dtypes.bfloat16)
    c["c_alk"] = alk.astype(ml_dtypes.bfloat16)
    return c


def make_in_maps(inputs, n_cores=8):
    consts = host_constants()
    maps = []
    for b in range(n_cores):
        m = {"x": np.ascontiguousarray(inputs["x"][b], dtype=np.float32)}
        for k, v in inputs.items():
            if k == "x":
                continue
            a = np.asarray(v, dtype=np.float32)
            if k == "router_bias":
                a = a.reshape(1, NE)
            m[k] = np.ascontiguousarray(a)
        m.update(consts)
        maps.append(m)
    return maps


def kernel(**inputs):
    nc, _, _ = build_program()
    maps = make_in_maps(inputs)
    res = run_bass_kernel_spmd(nc, maps, core_ids=list(range(8)))
    return np.stack([np.asarray(r["out"], dtype=np.float32) for r in res.results], axis=0)
```

```python
from contextlib import ExitStack
import numpy as np
import ml_dtypes
import concourse.bass as bass
import concourse.mybir as mybir
from concourse.bass_utils import run_bass_kernel_spmd

F32 = mybir.dt.float32
BF16 = mybir.dt.bfloat16
AF = mybir.ActivationFunctionType
ALU = mybir.AluOpType
AX = mybir.AxisListType

T = 2048
D = 1024
DEPTH = 2
NT = 16
NE = 16
DFF = 512
ALPHA = (2.0 * DEPTH) ** 0.25
LN_EPS = 1e-5
RMS_EPS = 1e-6
MLA_SCALE = 96.0 ** -0.5
NBIS = 16
BIG = 1.0e30
ENG_NAMES = ("pe", "act", "dve", "pool", "sp")


class Prog:
    def __init__(self, nc, st):
        self.nc = nc
        self.st = st
        self.esem = {e: st.enter_context(nc.semaphore("s_" + e)) for e in ENG_NAMES}
        self.ecount = {e: 0 for e in ENG_NAMES}
        self.csem = {}
        self.ccount = {}
        self.reset()

    def reset(self):
        self.ops = []
        self.res = {}

    def chan(self, name):
        if name not in self.csem:
            self.csem[name] = self.st.enter_context(self.nc.semaphore("c_" + name))
            self.ccount[name] = 0
        return name

    EXCL = ("pa", "pb", "tpb", "pc", "tp", "ST", "OB", "GA", "GB", "BCP", "TPM", "YP", "LGP", "HG", "HU", "YO")

    def op(self, eng, fn, r=(), w=(), chan=None):
        idx = len(self.ops)
        w = list(w) + [k for k in r if isinstance(k, str) and k.startswith(self.EXCL) and k not in w]
        deps = set()
        for k in r:
            e = self.res.get(k)
            if e is not None and e[0] is not None:
                deps.add(e[0])
        for k in w:
            e = self.res.get(k)
            if e is not None:
                if e[0] is not None:
                    deps.add(e[0])
                deps.update(e[1])
        deps.discard(idx)
        for k in r:
            e = self.res.setdefault(k, [None, []])
            e[1].append(idx)
        for k in w:
            self.res[k] = [idx, []]
        last = {}
        keep = set()
        for d in deps:
            o = self.ops[d]
            if o["chan"] is not None:
                keep.add(d)
            else:
                last[o["eng"]] = max(last.get(o["eng"], -1), d)
        deps = keep | set(last.values())
        dl = []
        for d in deps:
            o = self.ops[d]
            if o["chan"] is not None:
                dl.append(("c", o["chan"], self.ccount[o["chan"]]))
            else:
                dl.append(("e", d))
        cval = None
        if chan is not None:
            self.chan(chan)
            self.ccount[chan] += 16
            cval = self.ccount[chan]
        self.ops.append(dict(eng=eng, fn=fn, deps=dl, chan=chan, cval=cval, sig=False, sval=None))
        return idx

    def emit(self, final_wait=True):
        import os
        self.phase_idx = getattr(self, "phase_idx", -1) + 1
        skip = os.environ.get("SKIP_PHASES", "")
        if skip and str(self.phase_idx) in skip.split(","):
            self.reset()
            return
        nc = self.nc
        ops = self.ops
        for o in ops:
            for d in o["deps"]:
                if d[0] == "e":
                    p = ops[d[1]]
                    if p["eng"] != o["eng"] or o["eng"] != "pe" or o["chan"] is not None:
                        p["sig"] = True
        for o in ops:
            if o["chan"] is None and o["sig"]:
                self.ecount[o["eng"]] += 1
                o["sval"] = self.ecount[o["eng"]]
        per = {e: [] for e in ENG_NAMES}
        for o in ops:
            per[o["eng"]].append(o)
        chans_by_eng = {e: {} for e in ENG_NAMES}
        for o in ops:
            if o["chan"] is not None:
                chans_by_eng[o["eng"]][o["chan"]] = o["cval"]

        def replay(ename, eng):
            waited = {}
            for o in per[ename]:
                for d in o["deps"]:
                    if d[0] == "c":
                        sem, val, key = self.csem[d[1]], d[2], ("c", d[1])
                    else:
                        p = ops[d[1]]
                        if not p["sig"]:
                            continue
                        sem, val, key = self.esem[p["eng"]], p["sval"], ("e", p["eng"])
                    if waited.get(key, -1) >= val:
                        continue
                    waited[key] = val
                    eng.wait_ge(sem, val)
                ins = o["fn"](eng)
                if o["chan"] is not None:
                    ins.then_inc(self.csem[o["chan"]], 16)
                elif o["sig"]:
                    ins.then_inc(self.esem[ename], 1)
            if final_wait:
                for c, v in chans_by_eng[ename].items():
                    eng.wait_ge(self.csem[c], v)

        with nc.Block() as block:
            block.tensor(lambda e: replay("pe", e))
            block.scalar(lambda e: replay("act", e))
            block.vector(lambda e: replay("dve", e))
            block.gpsimd(lambda e: replay("pool", e))
            block.sync(lambda e: replay("sp", e))
        self.reset()


def sl(i, n=128):
    return slice(i * n, (i + 1) * n)


class TickQ:
    def __init__(self):
        self.q = []

    def push(self, delay, fn):
        self.q.append([delay, fn])

    def tick(self):
        for e in self.q:
            e[0] -= 1
        due = [e for e in self.q if e[0] <= 0]
        self.q = [e for e in self.q if e[0] > 0]
        for e in due:
            e[1]()

    def flush(self):
        while self.q:
            self.tick()


def emit_layernorm(P, pfx, ys, xo, gb, bb, st6, mv, key_in, key_out):
    P.op("dve", lambda e: e.bn_stats(out=st6[:, 0:6], in_=ys[:, 0:512]), r=[key_in], w=[pfx + "st6a"])
    P.op("dve", lambda e: e.bn_stats(out=st6[:, 6:12], in_=ys[:, 512:1024]), r=[key_in], w=[pfx + "st6b"])
    P.op("dve", lambda e: e.bn_aggr(out=mv[:, 0:2], in_=st6[:, 0:12]), r=[pfx + "st6a", pfx + "st6b"], w=[pfx + "mv"])
    P.op("dve", lambda e: e.tensor_scalar(out=mv[:, 2:3], in0=mv[:, 1:2], scalar1=LN_EPS, scalar2=None, op0=ALU.add),
         r=[pfx + "mv"], w=[pfx + "mv2"])
    P.op("act", lambda e: e.activation(out=mv[:, 3:4], in_=mv[:, 2:3], func=AF.Ln), r=[pfx + "mv2"], w=[pfx + "mv3"])
    P.op("act", lambda e: e.activation(out=mv[:, 4:5], in_=mv[:, 3:4], func=AF.Exp, scale=-0.5), r=[pfx + "mv3"], w=[pfx + "mv4"])
    P.op("dve", lambda e: e.tensor_scalar(out=ys[:, :], in0=ys[:, :], scalar1=mv[:, 0:1], scalar2=mv[:, 4:5],
                                         op0=ALU.subtract, op1=ALU.mult),
         r=[key_in, pfx + "mv", pfx + "mv4"], w=[key_in])
    P.op("pool", lambda e: e.tensor_tensor(out=ys[:, :], in0=ys[:, :], in1=gb[:, :], op=ALU.mult),
         r=[key_in, "lng"], w=[key_in])
    P.op("pool", lambda e: e.tensor_tensor(out=xo[:, :], in0=ys[:, :], in1=bb[:, :], op=ALU.add),
         r=[key_in, "lnb"], w=[key_out])


def emit_xt_update(P, xo, key_x, XT, tt, identf, tp0, tp1, extra=None):
    for half, tp in ((0, tp0), (1, tp1)):
        kb = "tp%d" % half
        for q in range(4):
            c = half * 4 + q
            P.op("pe", lambda e, c=c, q=q, tp=tp: e.transpose(out=tp[:, sl(q)], in_=xo[:, sl(c)], identity=identf[:, :]),
                 r=[key_x, "identf"], w=[kb])
        P.op("act", lambda e, half=half, tp=tp: e.activation(
            out=XT[:, half * 4:half * 4 + 4, sl(tt)], in_=tp[:, :].rearrange("p (c t) -> p c t", c=4), func=AF.Copy),
            r=[kb], w=[("XT", tt)])
        if extra is not None:
            extra(half, tp, kb)


def build_program(n_layers=DEPTH, stop_after=None, taps=None):
    nc = bass.Bass("TRN2", target_bir_lowering=False)
    dt_in = {}

    def din(name, shape, dt=F32):
        dt_in[name] = nc.dram_tensor(name, list(shape), dt, kind="ExternalInput").ap()
        return dt_in[name]

    x = din("x", [T, D])
    w_in = din("w_in", [DEPTH, D, 1608])
    q_norm_g = din("q_norm_g", [DEPTH, 384])
    w_q_up = din("w_q_up", [DEPTH, 384, 768])
    kv_norm_g = din("kv_norm_g", [DEPTH, 256])
    w_uk = din("w_uk", [DEPTH, 256, 512])
    w_uv = din("w_uv", [DEPTH, 256, 512])
    w_o = din("w_o", [DEPTH, D, D])
    ln1_g = din("ln1_g", [DEPTH, D])
    ln1_b = din("ln1_b", [DEPTH, D])
    router_w = din("router_w", [D, NE])
    router_bias = din("router_bias", [1, NE])
    w_gate = din("w_gate", [DEPTH, NE, D, DFF])
    w_up = din("w_up", [DEPTH, NE, D, DFF])
    w_down = din("w_down", [DEPTH, NE, DFF, D])
    ln2_g = din("ln2_g", [DEPTH, D])
    ln2_b = din("ln2_b", [DEPTH, D])
    c_identf = din("c_identf", [128, 128])
    c_identb = din("c_identb", [128, 128], BF16)
    c_cos = din("c_cos", [32, T])
    c_sin = din("c_sin", [32, T])
    c_tri = din("c_tri", [128, 128], BF16)
    c_triadd = din("c_triadd", [128, 128])
    c_alq = din("c_alq", [4, T], BF16)
    c_alk = din("c_alk", [4, T], BF16)

    out = nc.dram_tensor("out", [T, D], F32, kind="ExternalOutput").ap()
    xs1 = nc.dram_tensor("xs1", [T, D], F32, kind="Internal").ap()
    xs2 = nc.dram_tensor("xs2", [T, D], F32, kind="Internal").ap()
    tap_aps = {}
    if taps:
        for k, (shp, dt) in taps.items():
            tap_aps[k] = nc.dram_tensor("tap_" + k, list(shp), dt, kind="ExternalOutput").ap()

    st = ExitStack()
    sfx = [""]
    with st:
        P = Prog(nc, st)
        sb = lambda name, shape, dt=F32: st.enter_context(nc.sbuf_tensor(name + sfx[0], list(shape), dt))
        XT = sb("XT", [128, 8, T], BF16)
        MIXT = sb("MIXT", [128, 8, T], BF16)
        identf = sb("identf", [128, 128])
        identb = sb("identb", [128, 128], BF16)
        ones_f = sb("ones_f", [128, 128])
        GATES = sb("GATES", [128, NT, NE])

        def tap(P_, name, src_ap, key):
            if name in tap_aps:
                P_.op("sp", lambda e: e.dma_start(out=tap_aps[name], in_=src_ap), r=list(key), w=["tap_" + name],
                      chan="tap_" + name)

        with ExitStack() as ph:
            psb = lambda name, shape, dt=F32: ph.enter_context(nc.sbuf_tensor(name + sfx[0], list(shape), dt))
            pps = lambda name, shape, dt=F32: ph.enter_context(nc.psum_tensor(name + sfx[0], list(shape), dt))
            xin = [psb("p0_xin%d" % i, [128, D]) for i in range(2)]
            tp0 = pps("p0_tp0", [128, 512])
            tp1 = pps("p0_tp1", [128, 512])
            P.op("sp", lambda e: e.dma_start(out=identf[:, :], in_=c_identf), w=["identf"], chan="identf")
            P.op("sp", lambda e: e.dma_start(out=identb[:, :], in_=c_identb), w=["identb"], chan="identb")
            P.op("pool", lambda e: e.memset(ones_f[:, :], 1.0), w=["ones_f"])
            for tt in range(NT):
                b = tt % 2
                kx = "xin%d" % b
                P.op("sp", lambda e, tt=tt, b=b: e.dma_start(out=xin[b][:, :], in_=x[sl(tt), :]), w=[kx], chan=kx)
                emit_xt_update(P, xin[b], kx, XT, tt, identf, tp0, tp1)
            if stop_after == "p0" and "XT" in tap_aps:
                for tt in range(NT):
                    P.op("sp", lambda e, tt=tt: e.dma_start(out=tap_aps["XT"][:, :, sl(tt)], in_=XT[:, :, sl(tt)]),
                         r=[("XT", tt)], w=["tapXT%d" % tt], chan="tapXT")
            P.emit()

        for l in range(n_layers):
            sfx[0] = "_L%d" % l
            xsrc = x if l == 0 else xs2
            xdst2 = out if l == n_layers - 1 else xs2
            if stop_after == "p0":
                break
            mla = ExitStack()
            msb = lambda name, shape, dt=F32: mla.enter_context(nc.sbuf_tensor(name + sfx[0], list(shape), dt))
            QANT = msb("QANT", [128, 3, T], BF16)
            CKVT = msb("CKVT", [128, 2, T], BF16)
            KRT = msb("KRT", [96, T], BF16)
            COS = msb("COS", [96, T])
            SIN = msb("SIN", [96, T])
            with ExitStack() as ph:
                psb = lambda name, shape, dt=F32: ph.enter_context(nc.sbuf_tensor(name + sfx[0], list(shape), dt))
                pps = lambda name, shape, dt=F32: ph.enter_context(nc.psum_tensor(name + sfx[0], list(shape), dt))
                WM = psb("WM", [128, 8, 672], BF16)
                KSW = psb("KSW", [128, 8, 96], BF16)
                junk = psb("mi_junk", [128, 384])
                ss = [psb("mi_ss%d" % i, [128, 8]) for i in range(2)]
                qn = [psb("mi_qn%d" % i, [128, 640], BF16) for i in range(2)]
                rt = psb("mi_rt", [96, 2, 512])
                PA = [pps("mi_pa%d" % i, [128, 512]) for i in range(2)]
                PB = [pps("mi_pb%d" % i, [128, 512]) for i in range(2)]
                TPB = [pps("mi_tp%d" % i, [128, 1024], BF16) for i in range(2)]
                PC = [pps("mi_pc%d" % i, [128, 512]) for i in range(2)]
                wl = w_in[l].rearrange("(c p) n -> p c n", p=128)
                for c in range(8):
                    P.op("pool", lambda e, c=c: e.dma_start(out=WM[:, c, :], in_=wl[:, c, 0:672]), w=["WM"], chan="WM")
                P.op("dve", lambda e: e.memset(KSW[:, :, :], 0.0), w=["KSW"])
                P.op("pool", lambda e: e.dma_start(out=KSW[:, :, 64:80], in_=wl[:, :, 656:672]), r=[], w=["KSW"], chan="KSW")
                P.op("pool", lambda e: e.dma_start(out=KSW[:, :, 80:96], in_=wl[:, :, 640:656]), r=[], w=["KSW"], chan="KSW")
                P.op("sp", lambda e: e.dma_start(out=COS[64:96, :], in_=c_cos), w=["COS"], chan="COS")
                P.op("sp", lambda e: e.dma_start(out=SIN[64:96, :], in_=c_sin), w=["SIN"], chan="SIN")
                miq = TickQ()

                def mi_tile(tt):
                    b = tt % 2
                    pa, pb, tpb, s_, qn_ = PA[b], PB[b], TPB[b], ss[b], qn[b]
                    ka, kb_, kt, ks, kq = "pa%d" % b, "pb%d" % b, "tpb%d" % b, "ss%d" % b, "qn%d" % b
                    for c in range(8):
                        P.op("pe", lambda e, c=c, pa=pa: e.matmul(pa[:, :], lhsT=XT[:, c, sl(tt)], rhs=WM[:, c, 0:512],
                                                                    start=(c == 0), stop=(c == 7)),
                             r=[("XT", tt), "WM"], w=[ka])
                    for c in range(8):
                        P.op("pe", lambda e, c=c, pb=pb: e.matmul(pb[:, 0:128], lhsT=XT[:, c, sl(tt)], rhs=WM[:, c, 512:640],
                                                                    start=(c == 0), stop=(c == 7)),
                             r=[("XT", tt), "WM"], w=[kb_])
                    P.op("act", lambda e, pa=pa, s_=s_: e.activation(out=junk[:, 0:384], in_=pa[:, 0:384], func=AF.Square,
                                                                     accum_out=s_[:, 0:1]), r=[ka], w=[ks + "a", "junk"])
                    P.op("act", lambda e, pa=pa, s_=s_: e.activation(out=junk[:, 0:128], in_=pa[:, 384:512], func=AF.Square,
                                                                     accum_out=s_[:, 1:2]), r=[ka], w=[ks + "b", "junk"])
                    P.op("act", lambda e, pb=pb, s_=s_: e.activation(out=junk[:, 0:128], in_=pb[:, 0:128], func=AF.Square,
                                                                     accum_out=s_[:, 2:3]), r=[kb_], w=[ks + "c", "junk"])
                    P.op("dve", lambda e, s_=s_: e.tensor_scalar(out=s_[:, 3:4], in0=s_[:, 0:1], scalar1=1.0 / 384, scalar2=RMS_EPS,
                                                                 op0=ALU.mult, op1=ALU.add), r=[ks + "a"], w=[ks + "d"])
                    P.op("dve", lambda e, s_=s_: e.tensor_tensor(out=s_[:, 1:2], in0=s_[:, 1:2], in1=s_[:, 2:3], op=ALU.add),
                         r=[ks + "b", ks + "c"], w=[ks + "b"])
                    P.op("dve", lambda e, s_=s_: e.tensor_scalar(out=s_[:, 4:5], in0=s_[:, 1:2], scalar1=1.0 / 256, scalar2=RMS_EPS,
                                                                 op0=ALU.mult, op1=ALU.add), r=[ks + "b"], w=[ks + "e"])
                    P.op("act", lambda e, s_=s_: e.activation(out=s_[:, 5:7], in_=s_[:, 3:5], func=AF.Ln),
                         r=[ks + "d", ks + "e"], w=[ks + "f"])
                    P.op("act", lambda e, s_=s_: e.activation(out=s_[:, 3:5], in_=s_[:, 5:7], func=AF.Exp, scale=-0.5), r=[ks + "f"], w=[ks + "d", ks + "e"])
                    P.op("dve", lambda e, pa=pa, s_=s_, qn_=qn_: e.tensor_scalar(out=qn_[:, 0:384], in0=pa[:, 0:384], scalar1=s_[:, 3:4],
                                                                                scalar2=None, op0=ALU.mult), r=[ka, ks + "d"], w=[kq + "a"])
                    P.op("dve", lambda e, pa=pa, s_=s_, qn_=qn_: e.tensor_scalar(out=qn_[:, 384:512], in0=pa[:, 384:512], scalar1=s_[:, 4:5],
                                                                                scalar2=None, op0=ALU.mult), r=[ka, ks + "e"], w=[kq + "b"])
                    P.op("dve", lambda e, pb=pb, s_=s_, qn_=qn_: e.tensor_scalar(out=qn_[:, 512:640], in0=pb[:, 0:128], scalar1=s_[:, 4:5],
                                                                                scalar2=None, op0=ALU.mult), r=[kb_, ks + "e"], w=[kq + "c"])
                    def tail():
                        for k in range(5):
                            P.op("pe", lambda e, k=k, tpb=tpb, qn_=qn_: e.transpose(out=tpb[:, sl(k)], in_=qn_[:, sl(k)], identity=identb[:, :]),
                                 r=[kq + "a", kq + "b", kq + "c", "identb"], w=[kt])
                        P.op("act", lambda e, tpb=tpb: e.activation(out=QANT[:, :, sl(tt)],
                                                                    in_=tpb[:, 0:384].rearrange("p (c t) -> p c t", c=3), func=AF.Copy),
                             r=[kt], w=[("QANT", tt)])
                        P.op("act", lambda e, tpb=tpb: e.activation(out=CKVT[:, :, sl(tt)],
                                                                    in_=tpb[:, 384:640].rearrange("p (c t) -> p c t", c=2), func=AF.Copy),
                             r=[kt], w=[("CKVT", tt)])
                    miq.push(2, tail)
                import os as _os
                _lvl = int(_os.environ.get("MI_LEVEL", "9"))
                for tt in range(NT if _lvl >= 1 else 0):
                    mi_tile(tt)
                    miq.tick()
                miq.flush()

                def mi_kr(tc):
                    pc1, pc2 = PC[0], PC[1]
                    for c in range(8):
                        P.op("pe", lambda e, c=c: e.matmul(pc1[0:96, :], lhsT=WM[:, c, 576:672], rhs=XT[:, c, sl(tc, 512)],
                                                           start=(c == 0), stop=(c == 7)),
                             r=[("XT", 4 * tc + i) for i in range(4)] + ["WM"], w=["pc1"])
                    for c in range(8):
                        P.op("pe", lambda e, c=c: e.matmul(pc2[0:96, :], lhsT=KSW[:, c, 0:96], rhs=XT[:, c, sl(tc, 512)],
                                                           start=(c == 0), stop=(c == 7)),
                             r=[("XT", 4 * tc + i) for i in range(4)] + ["KSW"], w=["pc2"])
                    P.op("dve", lambda e: e.tensor_tensor(out=rt[64:96, 0, :], in0=pc1[64:96, :], in1=COS[64:96, sl(tc, 512)], op=ALU.mult),
                         r=["pc1", "COS"], w=["rt0"])
                    P.op("dve", lambda e: e.tensor_tensor(out=rt[64:96, 1, :], in0=pc2[64:96, :], in1=SIN[64:96, sl(tc, 512)], op=ALU.mult),
                         r=["pc2", "SIN"], w=["rt1"])
                    P.op("dve", lambda e: e.tensor_tensor(out=KRT[64:96, sl(tc, 512)], in0=rt[64:96, 0, :], in1=rt[64:96, 1, :], op=ALU.add),
                         r=["rt0", "rt1"], w=[("KRT", tc)])
                for tc in range(4 if _lvl >= 2 else 0):
                    mi_kr(tc)
                if stop_after == "mi":
                    tap(P, "QANT", QANT[:, :, :], [("QANT", i) for i in range(NT)])
                    tap(P, "CKVT", CKVT[:, :, :], [("CKVT", i) for i in range(NT)])
                    tap(P, "KRT", KRT[64:96, :], [("KRT", i) for i in range(4)])
                P.emit()
            if stop_after == "mi":
                mla.close()
                break
            with ExitStack() as ph:
                psb = lambda name, shape, dt=F32: ph.enter_context(nc.sbuf_tensor(name + sfx[0], list(shape), dt))
                pps = lambda name, shape, dt=F32: ph.enter_context(nc.psum_tensor(name + sfx[0], list(shape), dt))
                WQ = psb("WQ", [128, 3, 768], BF16)
                WQS = psb("WQS", [128, 3, 768], BF16)
                WUK = psb("WUK", [128, 2, 512], BF16)
                WUV = psb("WUV", [128, 2, 512], BF16)
                GQ = psb("GQ", [128, 3])
                GKV = psb("GKV", [128, 2])
                VA = psb("VA", [128, 16, 768], BF16)
                QTH = [psb("QTH%d" % i, [96, T], BF16) for i in range(2)]
                KTH = [psb("KTH%d" % i, [96, T], BF16) for i in range(2)]
                PT = [psb("PT%d" % i, [128, 512], BF16) for i in range(5)]
                OS = [psb("OS%d" % i, [128, 512]) for i in range(2)]
                LINV2 = [psb("LINV%d" % i, [128, 512]) for i in range(2)]
                rt2 = psb("rt2", [96, 2, 512])
                TRI = psb("TRI", [128, 128], BF16)
                ST = [pps("ST%d" % i, [128, 512]) for i in range(3)]
                OB = [pps("OB%d" % i, [128, 512]) for i in range(2)]
                GA = pps("GA", [128, 512])
                GB = pps("GB", [128, 512])
                BCP = pps("BCP", [128, 512])
                wq = w_q_up[l].rearrange("(c p) n -> p c n", p=128)
                P.op("sp", lambda e: e.dma_start(out=TRI[:, :], in_=c_tri), w=["TRI"], chan="TRI")
                P.op("pool", lambda e: e.dma_start(out=WQ[:, :, :], in_=wq), w=["WQ"], chan="WQ")
                P.op("dve", lambda e: e.memset(WQS[:, :, :], 0.0), w=["WQS"])
                wq4 = wq.rearrange("p c (h d) -> p c h d", d=96)
                WQS4 = WQS[:, :, :].rearrange("p c (h d) -> p c h d", d=96)
                for c in range(3):
                    P.op("pool", lambda e, c=c: e.dma_start(out=WQS4[:, c, :, 64:80], in_=wq4[:, c, :, 80:96]), w=["WQS"], chan="WQS")
                    P.op("pool", lambda e, c=c: e.dma_start(out=WQS4[:, c, :, 80:96], in_=wq4[:, c, :, 64:80]), w=["WQS"], chan="WQS")
                P.op("pool", lambda e: e.dma_start(out=WUK[:, :, :], in_=w_uk[l].rearrange("(c p) n -> p c n", p=128)), w=["WUK"], chan="WUK")
                P.op("pool", lambda e: e.dma_start(out=WUV[:, :, :], in_=w_uv[l].rearrange("(c p) n -> p c n", p=128)), w=["WUV"], chan="WUV")
                P.op("sp", lambda e: e.dma_start(out=GQ[:, :], in_=q_norm_g[l].rearrange("(c p) -> p c", p=128),
                                                 allow_slow_non_contiguous=True), w=["GQ"], chan="GQ")
                P.op("sp", lambda e: e.dma_start(out=GKV[:, :], in_=kv_norm_g[l].rearrange("(c p) -> p c", p=128),
                                                 allow_slow_non_contiguous=True), w=["GKV"], chan="GKV")
                for c in range(3):
                    P.op("dve", lambda e, c=c: e.tensor_scalar(out=WQ[:, c, :], in0=WQ[:, c, :], scalar1=GQ[:, c:c + 1], scalar2=None, op0=ALU.mult),
                         r=["GQ"], w=["WQ"])
                    P.op("dve", lambda e, c=c: e.tensor_scalar(out=WQS[:, c, :], in0=WQS[:, c, :], scalar1=GQ[:, c:c + 1], scalar2=None, op0=ALU.mult),
                         r=["GQ"], w=["WQS"])
                for c in range(2):
                    P.op("dve", lambda e, c=c: e.tensor_scalar(out=WUK[:, c, :], in0=WUK[:, c, :], scalar1=GKV[:, c:c + 1], scalar2=None, op0=ALU.mult),
                         r=["GKV"], w=["WUK"])
                    P.op("dve", lambda e, c=c: e.tensor_scalar(out=WUV[:, c, :], in0=WUV[:, c, :], scalar1=GKV[:, c:c + 1], scalar2=None, op0=ALU.mult),
                         r=["GKV"], w=["WUV"])
                P.op("pool", lambda e: e.memset(VA[:, :, :], 0.0), w=["VAc"])
                P.op("pool", lambda e: e.memset(VA[:, :, :].rearrange("p j (a x) -> p (j a) x", x=192)[:, :, 64:65], 1.0), w=["VAc"])

                def v_tile(j):
                    for c in range(2):
                        P.op("pe", lambda e, c=c: e.matmul(GA[:, :], lhsT=CKVT[:, c, sl(j)], rhs=WUV[:, c, :], start=(c == 0), stop=(c == 1)),
                             r=[("CKVT", j), "WUV"], w=["GA"])
                    src = GA[:, :].rearrange("p (a e d) -> p a e d", e=2, d=64)
                    dst = VA[:, j, :].rearrange("p (a x) -> p a x", x=192)
                    P.op("act", lambda e: e.activation(out=dst[:, :, 0:64], in_=src[:, :, 0, :], func=AF.Copy), r=["GA", "VAc"], w=[("VA", j)])
                    P.op("act", lambda e: e.activation(out=dst[:, :, 128:192], in_=src[:, :, 1, :], func=AF.Copy), r=["GA", "VAc"], w=[("VA", j)])
                for j in range(NT):
                    v_tile(j)
                cnt = [0]

                def norm_out(ob, obk, odd, dst, dstk, osb, osk, part, li):
                    lv, lk = LINV2[li], "LINV%d" % li
                    if not odd:
                        if part == 0:
                            P.op("act", lambda e: e.activation(out=osb[0:65, :], in_=ob[0:65, :], func=AF.Copy), r=[obk], w=[osk])
                            P.op("act", lambda e: e.activation(out=lv[64:65, :], in_=osb[64:65, :], func=AF.Ln), r=[osk], w=[lk])
                            P.op("act", lambda e: e.activation(out=lv[64:65, :], in_=lv[64:65, :], func=AF.Exp, scale=-1.0), w=[lk])
                        else:
                            P.op("pe", lambda e: e.matmul(BCP[0:64, :], lhsT=ones_f[64:65, 0:64], rhs=lv[64:65, :], start=True, stop=True),
                                 r=[lk, "ones_f"], w=["BCP"])
                            P.op("dve", lambda e: e.tensor_tensor(out=dst, in0=osb[0:64, :], in1=BCP[0:64, :], op=ALU.mult),
                                 r=[osk, "BCP"], w=[dstk])
                    else:
                        if part == 0:
                            P.op("act", lambda e: e.activation(out=osb[:, :], in_=ob[:, :], func=AF.Copy), r=[obk], w=[osk])
                            P.op("act", lambda e: e.activation(out=lv[0:1, :], in_=osb[0:1, :], func=AF.Ln), r=[osk], w=[lk])
                            P.op("act", lambda e: e.activation(out=lv[0:1, :], in_=lv[0:1, :], func=AF.Exp, scale=-1.0), w=[lk])
                        else:
                            P.op("pe", lambda e: e.matmul(BCP[:, :], lhsT=ones_f[0:1, :], rhs=lv[0:1, :], start=True, stop=True),
                                 r=[lk, "ones_f"], w=["BCP"])
                            P.op("dve", lambda e: e.tensor_tensor(out=dst, in0=osb[64:128, :], in1=BCP[64:128, :], op=ALU.mult),
                                 r=[osk, "BCP"], w=[dstk])

                def attn_chunk(tc, kt_fn, q_fn, v_fn, rk, odd, scale, mask_fn, ob, obk):
                    nj = 4 * tc + 4
                    M = 128 if odd else 65

                    def unit(j):
                        ip = j - 4 * tc
                        t0 = max(0, ip) * 128
                        N = 512 - t0
                        sb_ = cnt[0] % 3
                        pb_ = pcnt[0] % 5
                        cnt[0] += 1
                        pcnt[0] += 1
                        stt, stk, ptt, ptk = ST[sb_], "ST%d" % sb_, PT[pb_], "PT%d" % pb_
                        kap, qap, vap = kt_fn(j), q_fn(t0), v_fn(j)
                        P.op("pe", lambda e: e.matmul(stt[:, 0:N], lhsT=kap, rhs=qap, start=True, stop=True), r=rk, w=[stk])
                        P.op("act", lambda e: e.activation(out=ptt[:, 0:N], in_=stt[:, 0:N], func=AF.Exp, scale=scale), r=[stk], w=[ptk])
                        mask_fn(j, ip, t0, N, ptt, ptk)
                        pend.tick()
                        pend.push(3, lambda: P.op("pe", lambda e: e.matmul(ob[0:M, t0:512], lhsT=vap, rhs=ptt[:, 0:N], start=(j == 0), stop=(j == nj - 1)),
                                                  r=[ptk] + rk, w=[obk]))
                    for j in range(nj):
                        unit(j)

                pend = TickQ()
                ncnt = [0]
                pcnt = [0]

                def mla_gen(h):
                    b = h % 2
                    kth, qth = KTH[b], QTH[b]
                    kk, qk = "KTH%d" % b, "QTH%d" % b

                    def kgen(sc):
                        for c in range(2):
                            P.op("pe", lambda e, c=c: e.matmul(GA[0:64, :], lhsT=WUK[:, c, h * 64:(h + 1) * 64], rhs=CKVT[:, c, sl(sc, 512)],
                                                               start=(c == 0), stop=(c == 1)),
                                 r=[("CKVT", 4 * sc + i) for i in range(4)] + ["WUK"], w=["GA"])
                        P.op("dve", lambda e: e.tensor_copy(out=kth[0:64, sl(sc, 512)], in_=GA[0:64, :]), r=["GA"], w=[kk])
                    for sc in range(4):
                        kgen(sc)
                    P.op("pool", lambda e: e.tensor_copy(out=kth[64:96, :], in_=KRT[64:96, :]), r=[("KRT", i) for i in range(4)], w=[kk])

                    def qgen(tc):
                        for c in range(3):
                            P.op("pe", lambda e, c=c: e.matmul(GA[0:96, :], lhsT=WQ[:, c, h * 96:(h + 1) * 96], rhs=QANT[:, c, sl(tc, 512)],
                                                               start=(c == 0), stop=(c == 2)),
                                 r=[("QANT", 4 * tc + i) for i in range(4)] + ["WQ"], w=["GA"])
                        for c in range(3):
                            P.op("pe", lambda e, c=c: e.matmul(GB[0:96, :], lhsT=WQS[:, c, h * 96:(h + 1) * 96], rhs=QANT[:, c, sl(tc, 512)],
                                                               start=(c == 0), stop=(c == 2)),
                                 r=[("QANT", 4 * tc + i) for i in range(4)] + ["WQS"], w=["GB"])
                        P.op("dve", lambda e: e.tensor_copy(out=qth[0:64, sl(tc, 512)], in_=GA[0:64, :]), r=["GA"], w=[qk])
                        P.op("dve", lambda e: e.tensor_tensor(out=rt2[64:96, 0, :], in0=GA[64:96, :], in1=COS[64:96, sl(tc, 512)], op=ALU.mult),
                             r=["GA", "COS"], w=["rt20"])
                        P.op("dve", lambda e: e.tensor_tensor(out=rt2[64:96, 1, :], in0=GB[64:96, :], in1=SIN[64:96, sl(tc, 512)], op=ALU.mult),
                             r=["GB", "SIN"], w=["rt21"])
                        P.op("dve", lambda e: e.tensor_tensor(out=qth[64:96, sl(tc, 512)], in0=rt2[64:96, 0, :], in1=rt2[64:96, 1, :], op=ALU.add),
                             r=["rt20", "rt21"], w=[qk])
                    for tc in range(4):
                        qgen(tc)

                def mla_head(h):
                    b = h % 2
                    odd = (h % 2 == 1)
                    a = h // 2
                    kth, qth = KTH[b], QTH[b]
                    kk, qk = "KTH%d" % b, "QTH%d" % b

                    def mask_fn(j, ip, t0, N, ptt, ptk):
                        if ip >= 0:
                            P.op("dve", lambda e: e.tensor_tensor(out=ptt[:, 0:128], in0=ptt[:, 0:128], in1=TRI[:, :], op=ALU.mult),
                                 r=["TRI"], w=[ptk])
                    for tc in range(4):
                        ob, obk = OB[tc % 2], "OB%d" % (tc % 2)
                        osb, osk = OS[tc % 2], "OS%d" % (tc % 2)
                        attn_chunk(tc, lambda j: kth[0:96, sl(j)], lambda t0: qth[0:96, tc * 512 + t0:(tc + 1) * 512],
                                   (lambda j: VA[:, j, a * 192 + 64:a * 192 + 192]) if odd else (lambda j: VA[:, j, a * 192:a * 192 + 65]),
                                   [kk, qk] + [("VA", j) for j in range(4 * tc + 4)], odd, MLA_SCALE, mask_fn, ob, obk)
                        dst = MIXT[64:128, a, sl(tc, 512)] if odd else MIXT[0:64, a, sl(tc, 512)]
                        ncnt[0] += 1
                        li = ncnt[0] % 2
                        pend.push(4, lambda ob=ob, obk=obk, dst=dst, tc=tc, osb=osb, osk=osk, li=li: norm_out(ob, obk, odd, dst, ("MIXT", a, tc, odd), osb, osk, 0, li))
                        pend.push(8, lambda ob=ob, obk=obk, dst=dst, tc=tc, osb=osb, osk=osk, li=li: norm_out(ob, obk, odd, dst, ("MIXT", a, tc, odd), osb, osk, 1, li))
                mla_gen(0)
                for h in range(8):
                    if h + 1 < 8:
                        mla_gen(h + 1)
                    mla_head(h)
                pend.flush()
                if stop_after == "ma":
                    tap(P, "MIXA", MIXT[:, 0:4, :], [("MIXT", a, tc, o) for a in range(4) for tc in range(4) for o in (False, True)])
                P.emit()
            mla.close()
            if stop_after == "ma":
                break
            with ExitStack() as ph:
                psb = lambda name, shape, dt=F32: ph.enter_context(nc.sbuf_tensor(name + sfx[0], list(shape), dt))
                pps = lambda name, shape, dt=F32: ph.enter_context(nc.psum_tensor(name + sfx[0], list(shape), dt))
                WD = psb("WD", [128, 8, 936], BF16)
                IK3 = psb("IK3", [128, 8, 96], BF16)
                DKT = psb("DKT", [68, T], BF16)
                ALQ = psb("ALQ", [68, T], BF16)
                IQT = psb("IQT", [96, 3, T], BF16)
                IKT3 = psb("IKT3", [96, T], BF16)
                DVB = psb("DVB", [128, 16, 192], BF16)
                IW = psb("IW", [128, 16, 8])
                AW = psb("AW", [128, 16, 8])
                SG = psb("SG", [128, 16, 8])
                MT2 = [psb("MT%d" % i, [128, 16, 512], BF16) for i in range(2)]
                ACCs = [psb("ACC%d" % i, [128, T]) for i in range(2)]
                MKS = [psb("MK%d" % i, [128, T], BF16) for i in range(4)]
                JKs = [psb("JK0", [128, T], BF16)] * 2
                DQ = [psb("DQ%d" % i, [68, 512], BF16) for i in range(2)]
                PT = [psb("dPT%d" % i, [128, 512], BF16) for i in range(5)]
                OS = [psb("dOS%d" % i, [128, 512]) for i in range(3)]
                LINV2 = [psb("dLINV%d" % i, [128, 512]) for i in range(3)]
                JKA = psb("JKA", [128, T], BF16)
                TRI = psb("dTRI", [128, 128], BF16)
                TRIADD = psb("TRIADD", [128, 128])
                BSs = [psb("BS%d" % i, [128, 8]) for i in range(2)]
                POW = psb("POW", [128, NBIS + 1])
                WKTs = [psb("WKT%d" % i, [128, NBIS + 1]) for i in range(2)]
                WKT2s = [psb("WKT2_%d" % i, [128, NBIS + 1]) for i in range(2)]
                ST = [pps("dST%d" % i, [128, 512]) for i in range(3)]
                OB = [pps("dOB%d" % i, [128, 512]) for i in range(2)]
                GA = pps("dGA", [128, 512])
                BCP = pps("dBCP", [128, 512])
                TPM = pps("TPM", [128, 1024], BF16)
                wl = w_in[l].rearrange("(c p) n -> p c n", p=128)
                for c in range(8):
                    P.op("pool", lambda e, c=c: e.dma_start(out=WD[:, c, :], in_=wl[:, c, 672:1608]), w=["WD"], chan="WD")
                for k in range(3):
                    P.op("pool", lambda e, k=k: e.dma_start(out=IK3[:, :, 32 * k:32 * k + 32], in_=wl[:, :, 1568:1600]), w=["IK3"], chan="IK3")
                P.op("sp", lambda e: e.dma_start(out=TRI[:, :], in_=c_tri), w=["TRI"], chan="dTRI")
                P.op("sp", lambda e: e.dma_start(out=TRIADD[:, :], in_=c_triadd), w=["TRIADD"], chan="TRIADD")
                P.op("sp", lambda e: e.dma_start(out=DKT[64:68, :], in_=c_alk), w=["DKTc"], chan="DKTc")
                P.op("sp", lambda e: e.dma_start(out=ALQ[64:68, :], in_=c_alq), w=["ALQ"], chan="ALQ")
                for kk in range(NBIS + 1):
                    P.op("pool", lambda e, kk=kk: e.memset(POW[:, kk:kk + 1], 2.0 ** -(kk + 2)), w=["POW"])
                P.op("pool", lambda e: e.memset(DVB[:, :, :], 0.0), w=["DVBc"])
                P.op("pool", lambda e: e.memset(DVB[:, :, 64:65], 1.0), w=["DVBc"])

                def d_feat(tc):
                    xk = [("XT", 4 * tc + i) for i in range(4)]

                    def grp(c0, ncol, dst, dk_, wt=WD, wk="WD"):
                        for c in range(8):
                            P.op("pe", lambda e, c=c: e.matmul(GA[0:ncol, :], lhsT=wt[:, c, c0:c0 + ncol], rhs=XT[:, c, sl(tc, 512)],
                                                               start=(c == 0), stop=(c == 7)), r=xk + [wk], w=["GA"])
                        P.op("act", lambda e: e.activation(out=dst, in_=GA[0:ncol, :], func=AF.Copy), r=["GA"], w=[dk_])
                    grp(512, 64, DKT[0:64, sl(tc, 512)], ("DKT", tc))
                    grp(640, 96, IQT[0:96, 0, sl(tc, 512)], ("IQT", tc))
                    grp(736, 96, IQT[0:96, 1, sl(tc, 512)], ("IQT", tc))
                    grp(832, 64, IQT[0:64, 2, sl(tc, 512)], ("IQT", tc))
                    grp(0, 96, IKT3[0:96, sl(tc, 512)], ("IKT", tc), IK3, "IK3")
                for tc in range(4):
                    d_feat(tc)

                def d_tok(tt):
                    for c in range(8):
                        P.op("pe", lambda e, c=c: e.matmul(GA[:, 0:64], lhsT=XT[:, c, sl(tt)], rhs=WD[:, c, 576:640], start=(c == 0), stop=(c == 7)),
                             r=[("XT", tt), "WD"], w=["GA"])
                    for c in range(8):
                        P.op("pe", lambda e, c=c: e.matmul(GA[:, 64:72], lhsT=XT[:, c, sl(tt)], rhs=WD[:, c, 928:936], start=(c == 0), stop=(c == 7)),
                             r=[("XT", tt), "WD"], w=["GA"])
                    P.op("act", lambda e: e.activation(out=DVB[:, tt, 0:64], in_=GA[:, 0:64], func=AF.Copy), r=["GA", "DVBc"], w=[("DVB", tt)])
                    P.op("act", lambda e: e.activation(out=DVB[:, tt, 128:192], in_=GA[:, 0:64], func=AF.Copy), r=["GA", "DVBc"], w=[("DVB", tt)])
                    P.op("act", lambda e: e.activation(out=IW[:, tt, :], in_=GA[:, 64:72], func=AF.Copy), r=["GA"], w=["IW"])
                for tt in range(NT):
                    d_tok(tt)
                P.op("dve", lambda e: e.tensor_scalar(out=SG[:, :, :], in0=IW[:, :, :], scalar1=0.0, scalar2=2.0, op0=ALU.is_ge, op1=ALU.mult),
                     r=["IW"], w=["SG"])
                P.op("dve", lambda e: e.tensor_scalar(out=SG[:, :, :], in0=SG[:, :, :], scalar1=-1.0, scalar2=None, op0=ALU.add), w=["SG"])
                P.op("dve", lambda e: e.scalar_tensor_tensor(out=AW[:, :, :], in0=IW[:, :, :], scalar=1.0 / 16, in1=SG[:, :, :], op0=ALU.mult, op1=ALU.mult),
                     r=["IW", "SG"], w=["AW"])
                cnt = [0]
                ncnt = [0]
                pcnt = [0]
                pend = TickQ()

                def idx_pair(tiles, tc):
                    info = []
                    for slot, i in enumerate(tiles):
                        il = i - 4 * tc
                        nk = (i + 1) * 128
                        nsc = (nk + 511) // 512
                        ACC, BS, WKT, WKT2, JK = ACCs[slot], BSs[slot], WKTs[slot], WKT2s[slot], JKs[slot]
                        bk = "bis%d" % slot

                        def one(h, sc, i=i, nk=nk, ACC=ACC, slot=slot):
                            k, pbase = h // 3, 32 * (h % 3)
                            wdt = min(512, nk - sc * 512)
                            sb_ = cnt[0] % 3
                            cnt[0] += 1
                            stt, stk = ST[sb_], "ST%d" % sb_
                            P.op("pe", lambda e: e.matmul(stt[:, 0:wdt], lhsT=IQT[pbase:pbase + 32, k, sl(i)], rhs=IKT3[pbase:pbase + 32, sc * 512:sc * 512 + wdt],
                                                          start=True, stop=True),
                                 r=[("IQT", tc)] + [("IKT", q) for q in range(tc + 1)], w=[stk])
                            P.op("act", lambda e: e.activation(out=stt[:, 0:wdt], in_=stt[:, 0:wdt], func=AF.Relu, scale=AW[:, i, h:h + 1]),
                                 r=[stk, "AW"], w=[stk])
                            if h == 0:
                                P.op("dve", lambda e: e.tensor_scalar(out=ACC[:, sc * 512:sc * 512 + wdt], in0=stt[:, 0:wdt], scalar1=SG[:, i, 0:1], scalar2=None,
                                                                      op0=ALU.mult), r=[stk, "SG"], w=[("ACC", slot, sc)])
                            else:
                                P.op("dve", lambda e: e.scalar_tensor_tensor(out=ACC[:, sc * 512:sc * 512 + wdt], in0=stt[:, 0:wdt], scalar=SG[:, i, h:h + 1],
                                                                             in1=ACC[:, sc * 512:sc * 512 + wdt], op0=ALU.mult, op1=ALU.add),
                                     r=[stk, "SG"], w=[("ACC", slot, sc)])
                        for h in range(8):
                            for sc in range(nsc):
                                one(h, sc)
                                yield
                        acck = [("ACC", slot, sc) for sc in range(nsc)]

                        def prep(i=i, nk=nk, ACC=ACC, BS=BS, WKT=WKT, WKT2=WKT2, bk=bk, acck=acck, slot=slot):
                            P.op("dve", lambda e: e.tensor_reduce(out=BS[:, 0:1], in_=ACC[:, 0:nk], axis=AX.X, op=ALU.max, apply_absolute_value=True),
                                 r=acck, w=[bk])
                            P.op("dve", lambda e: e.tensor_scalar(out=BS[:, 1:2], in0=BS[:, 0:1], scalar1=2.0, scalar2=2.0, op0=ALU.mult, op1=ALU.add), w=[bk])
                            P.op("dve", lambda e: e.tensor_scalar(out=WKT[:, :], in0=POW[:, :], scalar1=BS[:, 1:2], scalar2=None, op0=ALU.mult), r=["POW"], w=[bk])
                            P.op("dve", lambda e: e.tensor_scalar(out=WKT2[:, :], in0=POW[:, :], scalar1=BS[:, 1:2], scalar2=2.0, op0=ALU.mult, op1=ALU.mult),
                                 r=["POW"], w=[bk])
                            P.op("dve", lambda e: e.memset(BS[:, 2:3], 0.0), w=[bk])
                            P.op("pool", lambda e: e.tensor_tensor(out=ACC[:, sl(i)], in0=ACC[:, sl(i)], in1=TRIADD[:, :], op=ALU.add),
                                 r=["TRIADD", bk], w=[("ACC", slot, i // 4)])
                        prep()
                        info.append((i, il, nk, acck, ACC, BS, WKT, WKT2, JK, bk, slot))

                    def bis(kk, t):
                        i, il, nk, acck, ACC, BS, WKT, WKT2, JK, bk, slot = t
                        if slot == 1:
                            P.op("act", lambda e: e.activation(out=JKA[:, 0:nk], in_=ACC[:, 0:nk], func=AF.Sign, bias=BS[:, 2:3], scale=1.0,
                                                               accum_out=BS[:, 5:6]), r=acck + [bk], w=[bk + "c", "JKa"])
                            P.op("dve", lambda e: e.tensor_scalar(out=BS[:, 6:7], in0=BS[:, 5:6], scalar1=float(512 - nk), scalar2=WKT2[:, kk:kk + 1],
                                                                  op0=ALU.is_lt, op1=ALU.mult), r=[bk + "c"], w=[bk])
                            P.op("dve", lambda e: e.scalar_tensor_tensor(out=BS[:, 2:3], in0=BS[:, 6:7], scalar=WKT[:, kk:kk + 1], in1=BS[:, 2:3],
                                                                         op0=ALU.subtract, op1=ALU.add), w=[bk])
                            return
                        P.op("dve", lambda e: e.tensor_scalar(out=JK[:, 0:nk], in0=ACC[:, 0:nk], scalar1=BS[:, 2:3], scalar2=None, op0=ALU.is_ge,
                                                              op1=ALU.add, accum_out=BS[:, 5:6]), r=acck, w=[bk, "JK%d" % slot])
                        P.op("dve", lambda e: e.tensor_scalar(out=BS[:, 6:7], in0=BS[:, 5:6], scalar1=256.0, scalar2=WKT2[:, kk:kk + 1],
                                                              op0=ALU.is_ge, op1=ALU.mult), w=[bk])
                        P.op("dve", lambda e: e.scalar_tensor_tensor(out=BS[:, 2:3], in0=BS[:, 6:7], scalar=WKT[:, kk:kk + 1], in1=BS[:, 2:3],
                                                                     op0=ALU.subtract, op1=ALU.add), w=[bk])
                    for kk in range(NBIS):
                        for t in info:
                            bis(kk, t)
                        yield

                    def fin(t):
                        i, il, nk, acck, ACC, BS, WKT, WKT2, JK, bk, slot = t
                        if slot == 1:
                            P.op("dve", lambda e: e.tensor_scalar(out=BS[:, 2:3], in0=BS[:, 2:3], scalar1=-1.0, scalar2=WKT2[:, NBIS:NBIS + 1],
                                                                  op0=ALU.mult, op1=ALU.subtract), w=[bk])
                        else:
                            P.op("dve", lambda e: e.tensor_tensor(out=BS[:, 2:3], in0=BS[:, 2:3], in1=WKT2[:, NBIS:NBIS + 1], op=ALU.subtract), w=[bk])
                        MK = MKS[il]
                        P.op("dve", lambda e: e.tensor_scalar(out=MK[:, 0:nk], in0=ACC[:, 0:nk], scalar1=BS[:, 2:3], scalar2=None, op0=ALU.is_ge),
                             r=acck + [bk], w=[("MK", il)])
                    for t in info:
                        fin(t)
                    yield

                def idx_steps(tiles):
                    return sum(8 * (((i + 1) * 128 + 511) // 512) for i in tiles) + NBIS + 1

                def idx_transposes(i, tc):
                    il = i - 4 * tc
                    MK = MKS[il]
                    MT = MT2[tc % 2]

                    def tgrp(j0, n):
                        for jj in range(n):
                            P.op("pe", lambda e, jj=jj: e.transpose(out=TPM[:, sl(jj)], in_=MK[:, sl(j0 + jj)], identity=identb[:, :]),
                                 r=[("MK", il), "identb"], w=["TPM"])
                        P.op("act", lambda e: e.activation(out=MT[:, j0:j0 + n, sl(il)], in_=TPM[:, 0:n * 128].rearrange("p (j t) -> p j t", t=128),
                                                           func=AF.Copy), r=["TPM"], w=[("MT", tc % 2, il)])
                    for j0 in range(0, i + 1, 8):
                        tgrp(j0, min(8, i + 1 - j0))

                def idx_special(i):
                    MT = MT2[0]
                    if i == 0:
                        P.op("pool", lambda e: e.tensor_copy(out=MT[:, 0, 0:128], in_=TRI[:, :]), r=["TRI"], w=[("MT", 0, 0)])
                    else:
                        P.op("pool", lambda e: e.memset(MT[:, 0, 128:256], 1.0), w=[("MT", 0, 1)])
                        P.op("pool", lambda e: e.tensor_copy(out=MT[:, 1, 128:256], in_=TRI[:, :]), r=["TRI"], w=[("MT", 0, 1)])

                def dsa_chunk(tc):
                    MT = MT2[tc % 2]
                    mtk = [("MT", tc % 2, q) for q in range(4)]
                    nxt = [i for i in range(4 * tc + 4, 4 * tc + 8)] if tc < 3 else []
                    import itertools, math
                    if nxt:
                        gen = itertools.chain(idx_pair(nxt[0:2], tc + 1), idx_pair(nxt[2:4], tc + 1))
                        rate = int(math.ceil((idx_steps(nxt[0:2]) + idx_steps(nxt[2:4])) / float(8 * (4 * tc + 4) - 8)))
                    else:
                        gen, rate = iter(()), 0

                    def dqgen(h):
                        b = h % 2
                        dq, dqk = DQ[b], "DQ%d" % b
                        for c in range(8):
                            P.op("pe", lambda e, c=c: e.matmul(GA[0:64, :], lhsT=WD[:, c, h * 64:(h + 1) * 64], rhs=XT[:, c, sl(tc, 512)],
                                                               start=(c == 0), stop=(c == 7)),
                                 r=[("XT", 4 * tc + q) for q in range(4)] + ["WD"], w=["GA"])
                        P.op("act", lambda e: e.mul(out=dq[0:64, :], in_=GA[0:64, :], mul=0.125 * 2.0 ** (h + 1)), r=["GA"], w=[dqk])
                        P.op("pool", lambda e: e.tensor_copy(out=dq[64:68, :], in_=ALQ[64:68, sl(tc, 512)]), r=["ALQ"], w=[dqk])

                    def head(h):
                        b = h % 2
                        odd = (h % 2 == 1)
                        dq, dqk = DQ[b], "DQ%d" % b
                        ucount = [0]

                        def mask_fn(j, ip, t0, N, ptt, ptk):
                            P.op("pool", lambda e: e.tensor_tensor(out=ptt[:, 0:N], in0=ptt[:, 0:N], in1=MT[:, j, t0:512], op=ALU.mult),
                                 r=mtk, w=[ptk])
                            for _ in range(rate):
                                next(gen, None)
                        ob, obk = OB[h % 2], "OB%d" % (h % 2)
                        osb, osk = OS[ncnt[0] % 3], "OS%d" % (ncnt[0] % 3)
                        attn_chunk2(tc, lambda j: DKT[0:68, sl(j)], lambda t0: dq[0:68, t0:512],
                                    (lambda j: DVB[:, j, 64:192]) if odd else (lambda j: DVB[:, j, 0:65]),
                                    [dqk, "DKTc"] + [("DKT", q) for q in range(tc + 1)] + [("DVB", j) for j in range(4 * tc + 4)],
                                    odd, 2.0 ** -(h + 1), mask_fn, ob, obk)
                        a = 4 + h // 2
                        dst = MIXT[64:128, a, sl(tc, 512)] if odd else MIXT[0:64, a, sl(tc, 512)]
                        li = ncnt[0] % 3
                        ncnt[0] += 1
                        pend.push(4, lambda: norm_out2(ob, obk, odd, dst, ("MIXT", a, tc, odd), osb, osk, 0, li))
                        pend.push(8, lambda: norm_out2(ob, obk, odd, dst, ("MIXT", a, tc, odd), osb, osk, 1, li))
                    dqgen(0)
                    for h in range(8):
                        if h + 1 < 8:
                            dqgen(h + 1)
                        head(h)
                    for _ in gen:
                        pass
                    pend.flush()
                    for i in nxt:
                        idx_transposes(i, tc + 1)

                def norm_out2(ob, obk, odd, dst, dstk, osb, osk, part, li):
                    lv, lk = LINV2[li], "LINV%d" % li
                    if not odd:
                        if part == 0:
                            P.op("act", lambda e: e.activation(out=osb[0:65, :], in_=ob[0:65, :], func=AF.Copy), r=[obk], w=[osk])
                            P.op("act", lambda e: e.activation(out=lv[64:65, :], in_=osb[64:65, :], func=AF.Ln), r=[osk], w=[lk])
                            P.op("act", lambda e: e.activation(out=lv[64:65, :], in_=lv[64:65, :], func=AF.Exp, scale=-1.0), w=[lk])
                        else:
                            P.op("pe", lambda e: e.matmul(BCP[0:64, :], lhsT=ones_f[64:65, 0:64], rhs=lv[64:65, :], start=True, stop=True),
                                 r=[lk, "ones_f"], w=["BCP"])
                            P.op("dve", lambda e: e.tensor_tensor(out=dst, in0=osb[0:64, :], in1=BCP[0:64, :], op=ALU.mult),
                                 r=[osk, "BCP"], w=[dstk])
                    else:
                        if part == 0:
                            P.op("act", lambda e: e.activation(out=osb[:, :], in_=ob[:, :], func=AF.Copy), r=[obk], w=[osk])
                            P.op("act", lambda e: e.activation(out=lv[0:1, :], in_=osb[0:1, :], func=AF.Ln), r=[osk], w=[lk])
                            P.op("act", lambda e: e.activation(out=lv[0:1, :], in_=lv[0:1, :], func=AF.Exp, scale=-1.0), w=[lk])
                        else:
                            P.op("pe", lambda e: e.matmul(BCP[:, :], lhsT=ones_f[0:1, :], rhs=lv[0:1, :], start=True, stop=True),
                                 r=[lk, "ones_f"], w=["BCP"])
                            P.op("dve", lambda e: e.tensor_tensor(out=dst, in0=osb[64:128, :], in1=BCP[64:128, :], op=ALU.mult),
                                 r=[osk, "BCP"], w=[dstk])

                def attn_chunk2(tc, kt_fn, q_fn, v_fn, rk, odd, scale, mask_fn, ob, obk):
                    nj = 4 * tc + 4
                    M = 128 if odd else 65

                    def unit(j):
                        ip = j - 4 * tc
                        t0 = max(0, ip) * 128
                        N = 512 - t0
                        sb_ = cnt[0] % 3
                        pb_ = pcnt[0] % 5
                        cnt[0] += 1
                        pcnt[0] += 1
                        stt, stk, ptt, ptk = ST[sb_], "ST%d" % sb_, PT[pb_], "PT%d" % pb_
                        kap, qap, vap = kt_fn(j), q_fn(t0), v_fn(j)
                        P.op("pe", lambda e: e.matmul(stt[:, 0:N], lhsT=kap, rhs=qap, start=True, stop=True), r=rk, w=[stk])
                        P.op("act", lambda e: e.activation(out=ptt[:, 0:N], in_=stt[:, 0:N], func=AF.Exp, scale=scale), r=[stk], w=[ptk])
                        mask_fn(j, ip, t0, N, ptt, ptk)
                        pend.tick()
                        pend.push(3, lambda: P.op("pe", lambda e: e.matmul(ob[0:M, t0:512], lhsT=vap, rhs=ptt[:, 0:N], start=(j == 0), stop=(j == nj - 1)),
                                                  r=[ptk] + rk, w=[obk]))
                    for j in range(nj):
                        unit(j)
                idx_special(0)
                idx_special(1)
                for _ in idx_pair([2, 3], 0):
                    pass
                idx_transposes(2, 0)
                idx_transposes(3, 0)
                for tc in range(4):
                    dsa_chunk(tc)
                if stop_after == "da":
                    tap(P, "MIXB", MIXT[:, 4:8, :], [("MIXT", a, tc, o) for a in range(4, 8) for tc in range(4) for o in (False, True)])
                P.emit()
            if stop_after == "da":
                break
            pre_st = ExitStack()
            PW = [pre_st.enter_context(nc.sbuf_tensor(n_ + sfx[0], shp_, BF16))
                  for n_, shp_ in (("WG0", [128, 8, DFF]), ("WU0", [128, 8, DFF]), ("WDN0", [128, 4, D]))]
            with ExitStack() as ph:
                psb = lambda name, shape, dt=F32: ph.enter_context(nc.sbuf_tensor(name + sfx[0], list(shape), dt))
                pps = lambda name, shape, dt=F32: ph.enter_context(nc.psum_tensor(name + sfx[0], list(shape), dt))
                WO = psb("WO", [128, 8, D], BF16)
                LG_ = psb("LNG", [128, D])
                LB_ = psb("LNB", [128, D])
                RW = psb("RW", [128, 8, NE])
                RB = psb("RB", [128, NE])
                xin = [psb("w_xin%d" % i, [128, D]) for i in range(2)]
                ys = [psb("w_ys%d" % i, [128, D]) for i in range(2)]
                xo = [psb("w_xo%d" % i, [128, D]) for i in range(2)]
                XRs = [psb("XR%d" % i, [128, 8, 128]) for i in range(2)]
                wq_ = TickQ()
                st6 = psb("w_st6", [128, 12])
                mv = psb("w_mv", [128, 8])
                rs = psb("w_rs", [128, 12, NE])
                YP = [pps("YP%d" % i, [128, D]) for i in range(2)]
                tp0 = pps("w_tp0", [128, 512])
                tp1 = pps("w_tp1", [128, 512])
                LGP = pps("LGP", [128, 512])
                wo = w_o[l].rearrange("(c p) n -> p c n", p=128)
                for c in range(8):
                    P.op("pool", lambda e, c=c: e.dma_start(out=WO[:, c, :], in_=wo[:, c, :]), w=["WO"], chan="WO")
                P.op("pool", lambda e: e.dma_start(out=PW[0][:, :, :], in_=w_gate[l, 0].rearrange("(c p) n -> p c n", p=128)), w=["PWG0"], chan="WG0")
                P.op("pool", lambda e: e.dma_start(out=PW[1][:, :, :], in_=w_up[l, 0].rearrange("(c p) n -> p c n", p=128)), w=["PWU0"], chan="WU0")
                P.op("pool", lambda e: e.dma_start(out=PW[2][:, :, :], in_=w_down[l, 0].rearrange("(c p) n -> p c n", p=128)), w=["PWDN0"], chan="WDN0")
                P.op("sp", lambda e: e.dma_start(out=LG_[:, :], in_=ln1_g[l:l + 1, :].to_broadcast([128, D])), w=["lng"], chan="lng")
                P.op("sp", lambda e: e.dma_start(out=LB_[:, :], in_=ln1_b[l:l + 1, :].to_broadcast([128, D])), w=["lnb"], chan="lnb")
                P.op("sp", lambda e: e.dma_start(out=RW[:, :, :], in_=router_w.rearrange("(c p) n -> p c n", p=128)), w=["RW"], chan="RW")
                P.op("sp", lambda e: e.dma_start(out=RB[:, :], in_=router_bias[0:1, :].to_broadcast([128, NE])), w=["RB"], chan="RB")

                import os as _os
                _wl = int(_os.environ.get("W_LEVEL", "9"))

                def w_tile(tt):
                    b = tt % 2
                    xi, xik, y, yk, xo_, xok, yp, ypk = xin[b], "xin%d" % b, ys[b], "ys%d" % b, xo[b], "xo%d" % b, YP[b], "YP%d" % b
                    if _wl < 1:
                        return
                    P.op("sp", lambda e: e.dma_start(out=xi[:, :], in_=xsrc[sl(tt), :]), w=[xik], chan=xik)
                    for n in range(2):
                        for c in range(8):
                            P.op("pe", lambda e, n=n, c=c: e.matmul(yp[:, sl(n, 512)], lhsT=MIXT[:, c, sl(tt)], rhs=WO[:, c, sl(n, 512)],
                                                                    start=(c == 0), stop=(c == 7)),
                                 r=[("MIXT", a, tt // 4, o) for a in range(8) for o in (False, True)] + ["WO"], w=[ypk])
                    P.op("dve", lambda e: e.scalar_tensor_tensor(out=y[:, :], in0=xi[:, :], scalar=ALPHA, in1=yp[:, :], op0=ALU.mult, op1=ALU.add),
                         r=[xik, ypk], w=[yk])
                    emit_layernorm(P, "w", y, xo_, LG_, LB_, st6, mv, yk, xok)
                    if _wl < 2:
                        return
                    P.op("pool", lambda e: e.dma_start(out=xs1[sl(tt), :], in_=xo_[:, :]), r=[xok], w=["xs1_%d" % tt], chan="xs1w%d" % b)
                    if _wl < 3:
                        return

                    XR = XRs[b]

                    def extra(half, tp, kb):
                        P.op("act", lambda e: e.activation(out=XR[:, half * 4:half * 4 + 4, :], in_=tp[:, :].rearrange("p (c t) -> p c t", c=4), func=AF.Copy),
                             r=[kb], w=["XR%d_%d" % (b, half)])
                    wq_.push(2, lambda: emit_xt_update(P, xo_, xok, XT, tt, identf, tp0, tp1, extra))
                    wq_.push(3, lambda: w_router(tt, b))

                def w_router(tt, b):
                    XR = XRs[b]
                    for c in range(8):
                        P.op("pe", lambda e, c=c: e.matmul(LGP[:, 0:NE], lhsT=XR[:, c, :], rhs=RW[:, c, :], start=(c == 0), stop=(c == 7)),
                             r=["XR%d_0" % b, "XR%d_1" % b, "RW"], w=["LGP"])
                    A, Bi = rs[:, 0, :], rs[:, 1, :]
                    k = "rs"
                    P.op("act", lambda e: e.activation(out=A, in_=LGP[:, 0:NE], func=AF.Exp, scale=-1.0), r=["LGP"], w=[k])
                    P.op("dve", lambda e: e.tensor_scalar(out=A, in0=A, scalar1=1.0, scalar2=None, op0=ALU.add), w=[k])
                    P.op("dve", lambda e: e.reciprocal(out=A, in_=A), w=[k])
                    P.op("dve", lambda e: e.tensor_tensor(out=Bi, in0=A, in1=RB[:, :], op=ALU.add), r=["RB"], w=[k])
                    B4 = rs[:, 1, :].rearrange("p (g x) -> p g x", x=4)
                    m1, n1, m2, n2 = rs[:, 2, 0:4], rs[:, 2, 4:8], rs[:, 2, 8:12], rs[:, 2, 12:16]
                    P.op("dve", lambda e: e.tensor_tensor(out=m1, in0=B4[:, :, 0], in1=B4[:, :, 1], op=ALU.max), w=[k])
                    P.op("dve", lambda e: e.tensor_tensor(out=n1, in0=B4[:, :, 0], in1=B4[:, :, 1], op=ALU.min), w=[k])
                    P.op("dve", lambda e: e.tensor_tensor(out=m2, in0=B4[:, :, 2], in1=B4[:, :, 3], op=ALU.max), w=[k])
                    P.op("dve", lambda e: e.tensor_tensor(out=n2, in0=B4[:, :, 2], in1=B4[:, :, 3], op=ALU.min), w=[k])
                    t1, t2, t3, gs = rs[:, 3, 0:4], rs[:, 3, 4:8], rs[:, 3, 8:12], rs[:, 3, 12:16]
                    P.op("dve", lambda e: e.tensor_tensor(out=t1, in0=m1, in1=m2, op=ALU.max), w=[k])
                    P.op("dve", lambda e: e.tensor_tensor(out=t2, in0=m1, in1=m2, op=ALU.min), w=[k])
                    P.op("dve", lambda e: e.tensor_tensor(out=t3, in0=n1, in1=n2, op=ALU.max), w=[k])
                    P.op("dve", lambda e: e.tensor_tensor(out=t2, in0=t2, in1=t3, op=ALU.max), w=[k])
                    P.op("dve", lambda e: e.tensor_tensor(out=gs, in0=t1, in1=t2, op=ALU.add), w=[k])
                    gm = rs[:, 4, 0:1]
                    P.op("dve", lambda e: e.tensor_reduce(out=gm, in_=gs, axis=AX.X, op=ALU.max), w=[k])
                    gmask = rs[:, 4, 4:8]
                    P.op("dve", lambda e: e.tensor_scalar(out=gmask, in0=gs, scalar1=gm, scalar2=None, op0=ALU.is_ge), w=[k])
                    IG = rs[:, 5, :]
                    IG4 = rs[:, 5, :].rearrange("p (g x) -> p g x", x=4)
                    for xx in range(4):
                        P.op("dve", lambda e, xx=xx: e.tensor_copy(out=IG4[:, :, xx], in_=gmask), w=[k])
                    MB = rs[:, 6, :]
                    P.op("dve", lambda e: e.scalar_tensor_tensor(out=MB, in0=Bi, scalar=4.0, in1=IG, op0=ALU.add, op1=ALU.mult), w=[k])
                    tm = rs[:, 4, 1:2]
                    S1, S2 = rs[:, 7, :], rs[:, 8, :]
                    P.op("dve", lambda e: e.tensor_reduce(out=tm, in_=MB, axis=AX.X, op=ALU.max), w=[k])
                    P.op("dve", lambda e: e.tensor_scalar(out=S1, in0=MB, scalar1=tm, scalar2=None, op0=ALU.is_ge), w=[k])
                    P.op("dve", lambda e: e.scalar_tensor_tensor(out=MB, in0=S1, scalar=-8.0, in1=MB, op0=ALU.mult, op1=ALU.add), w=[k])
                    P.op("dve", lambda e: e.tensor_reduce(out=tm, in_=MB, axis=AX.X, op=ALU.max), w=[k])
                    P.op("dve", lambda e: e.tensor_scalar(out=S2, in0=MB, scalar1=tm, scalar2=None, op0=ALU.is_ge), w=[k])
                    P.op("dve", lambda e: e.tensor_tensor(out=S1, in0=S1, in1=S2, op=ALU.add), w=[k])
                    WT = rs[:, 9, :]
                    P.op("dve", lambda e: e.tensor_tensor(out=WT, in0=A, in1=S1, op=ALU.mult), w=[k])
                    ws = rs[:, 4, 2:3]
                    P.op("dve", lambda e: e.tensor_reduce(out=ws, in_=WT, axis=AX.X, op=ALU.add), w=[k])
                    P.op("dve", lambda e: e.reciprocal(out=ws, in_=ws), w=[k])
                    P.op("dve", lambda e: e.tensor_scalar(out=GATES[:, tt, :], in0=WT, scalar1=ws, scalar2=None, op0=ALU.mult), w=[k, ("GATES", tt)])
                for tt in range(NT):
                    w_tile(tt)
                    wq_.tick()
                wq_.flush()
                if stop_after == "w":
                    tap(P, "GATES", GATES[:, :, :], [("GATES", i) for i in range(NT)])
                P.emit()
            if stop_after == "w":
                break
            with ExitStack() as ph:
                psb = lambda name, shape, dt=F32: ph.enter_context(nc.sbuf_tensor(name + sfx[0], list(shape), dt))
                pps = lambda name, shape, dt=F32: ph.enter_context(nc.psum_tensor(name + sfx[0], list(shape), dt))
                MACC = psb("MACC", [128, NT, D])
                WG = [PW[0], psb("WG1", [128, 8, DFF], BF16)]
                WU = [PW[1], psb("WU1", [128, 8, DFF], BF16)]
                WDN = [PW[2], psb("WDN1", [128, 4, D], BF16)]
                AT = [psb("AT%d" % i, [128, 4, 512], BF16) for i in range(2)]
                SGT = [psb("SGT%d" % i, [128, 512]) for i in range(2)]
                LG_ = psb("LNG2", [128, D])
                LB_ = psb("LNB2", [128, D])
                xin = [psb("e_xin%d" % i, [128, D]) for i in range(2)]
                xo = xin
                st6 = psb("e_st6", [128, 12])
                mv = psb("e_mv", [128, 8])
                HG = [pps("HG%d" % i, [128, 512]) for i in range(2)]
                HU = [pps("HU%d" % i, [128, 512]) for i in range(2)]
                YO = [pps("YO%d" % i, [128, 512]) for i in range(2)]
                tp0 = pps("e_tp0", [128, 512])
                tp1 = pps("e_tp1", [128, 512])
                P.op("sp", lambda e: e.dma_start(out=LG_[:, :], in_=ln2_g[l:l + 1, :].to_broadcast([128, D])), w=["lng"], chan="lng2")
                P.op("sp", lambda e: e.dma_start(out=LB_[:, :], in_=ln2_b[l:l + 1, :].to_broadcast([128, D])), w=["lnb"], chan="lnb2")
                cnt = [0]

                eq = TickQ()

                def expert(ex):
                    b = ex % 2
                    wg, wu, wd = WG[b], WU[b], WDN[b]
                    kg, ku, kd = "WG%d" % b, "WU%d" % b, "WDN%d" % b
                    import os as _os
                    _md = _os.environ.get("MOE_DBG", "")
                    if "hwdge" in _md:
                        q_ = "pool" if "swq" in _md else "sp"
                        P.op(q_, lambda e: e.dma_start(out=wg[:, :, :], in_=w_gate[l, ex].bitcast(BF16).rearrange("(c p) n -> p c n", p=128)[:, :, 0:512]), w=[kg], chan=kg)
                        P.op(q_, lambda e: e.dma_start(out=wu[:, :, :], in_=w_up[l, ex].bitcast(BF16).rearrange("(c p) n -> p c n", p=128)[:, :, 0:512]), w=[ku], chan=ku)
                        P.op(q_, lambda e: e.dma_start(out=wd[:, :, :], in_=w_down[l, ex].bitcast(BF16).rearrange("(c p) n -> p c n", p=128)[:, :, 0:1024]), w=[kd], chan=kd)
                    elif ex == 0:
                        pass
                    elif "nodma" not in _md or ex < 2:
                        P.op("pool", lambda e: e.dma_start(out=wg[:, :, :], in_=w_gate[l, ex].rearrange("(c p) n -> p c n", p=128)), w=[kg], chan=kg)
                        P.op("pool", lambda e: e.dma_start(out=wu[:, :, :], in_=w_up[l, ex].rearrange("(c p) n -> p c n", p=128)), w=[ku], chan=ku)
                        P.op("pool", lambda e: e.dma_start(out=wd[:, :, :], in_=w_down[l, ex].rearrange("(c p) n -> p c n", p=128)), w=[kd], chan=kd)

                    def chunk(tc):
                        ab = cnt[0] % 2
                        cnt[0] += 1
                        at, atk = AT[ab], "AT%d" % ab
                        xk = [("XT", 4 * tc + q) for q in range(4)]

                        def fchunk(fc):
                            hb = fc % 2
                            hg, hu, sg = HG[hb], HU[hb], SGT[hb]
                            for c in range(8):
                                P.op("pe", lambda e, c=c: e.matmul(hg[:, :], lhsT=wg[:, c, sl(fc)], rhs=XT[:, c, sl(tc, 512)], start=(c == 0), stop=(c == 7)),
                                     r=xk + [kg], w=["HG%d" % hb])
                            for c in range(8):
                                P.op("pe", lambda e, c=c: e.matmul(hu[:, :], lhsT=wu[:, c, sl(fc)], rhs=XT[:, c, sl(tc, 512)], start=(c == 0), stop=(c == 7)),
                                     r=xk + [ku], w=["HU%d" % hb])
                            if "noact" in _md:
                                return
                            P.op("act", lambda e: e.activation(out=sg[:, :], in_=hg[:, :], func=AF.Silu), r=["HG%d" % hb], w=["SGT%d" % hb])
                            P.op("dve", lambda e: e.tensor_tensor(out=at[:, fc, :], in0=sg[:, :], in1=hu[:, :], op=ALU.mult),
                                 r=["SGT%d" % hb, "HU%d" % hb], w=[atk])
                        for fc in range(4):
                            fchunk(fc)

                        def down(tl, n):
                            tt = 4 * tc + tl
                            yb = (tl * 2 + n) % 2
                            yo = YO[yb]
                            for fc in range(4):
                                P.op("pe", lambda e, fc=fc: e.matmul(yo[:, :], lhsT=at[:, fc, sl(tl)], rhs=wd[:, fc, sl(n, 512)], start=(fc == 0), stop=(fc == 3)),
                                     r=[atk, kd], w=["YO%d" % yb])
                            if "noevac" in _md:
                                return
                            if ex == 0:
                                P.op("dve", lambda e: e.tensor_scalar(out=MACC[:, tt, sl(n, 512)], in0=yo[:, :], scalar1=GATES[:, tt, ex:ex + 1], scalar2=None,
                                                                      op0=ALU.mult), r=["YO%d" % yb, ("GATES", tt)], w=[("MACC", tt, n)])
                            else:
                                P.op("dve", lambda e: e.scalar_tensor_tensor(out=MACC[:, tt, sl(n, 512)], in0=yo[:, :], scalar=GATES[:, tt, ex:ex + 1],
                                                                             in1=MACC[:, tt, sl(n, 512)], op0=ALU.mult, op1=ALU.add),
                                     r=["YO%d" % yb, ("GATES", tt)], w=[("MACC", tt, n)])
                        def downs():
                            for tl in range(4):
                                for n in range(2):
                                    down(tl, n)
                            if ex == NE - 1:
                                for tl in range(4):
                                    e_tile(4 * tc + tl)
                        eq.push(2, downs)
                    return chunk

                def e_tile(tt):
                    b = tt % 2
                    xi, xik, xo_, xok = xin[b], "xin%d" % b, xo[b], "xin%d" % b
                    P.op("sp", lambda e: e.dma_start(out=xi[:, :], in_=xs1[sl(tt), :]), w=[xik], chan="e" + xik)
                    P.op("dve", lambda e: e.scalar_tensor_tensor(out=xi[:, :], in0=xi[:, :], scalar=ALPHA, in1=MACC[:, tt, :], op0=ALU.mult, op1=ALU.add),
                         r=[("MACC", tt, 0), ("MACC", tt, 1)], w=[xik])
                    emit_layernorm(P, "e", xi, xo_, LG_, LB_, st6, mv, xik, xok)
                    P.op("pool", lambda e: e.dma_start(out=xdst2[sl(tt), :], in_=xo_[:, :]), r=[xok], w=["xd2_%d" % tt], chan="xd2w%d" % b)
                    if l < n_layers - 1:
                        emit_xt_update(P, xo_, xok, XT, tt, identf, tp0, tp1)
                for ex in range(NE - 2):
                    ch = expert(ex)
                    for tc in range(4):
                        ch(tc)
                        eq.tick()
                cha = expert(NE - 2)
                chb = None
                for tc in range(4):
                    cha(tc)
                    eq.tick()
                    if chb is None:
                        chb = expert(NE - 1)
                    chb(tc)
                    eq.tick()
                eq.flush()
                P.emit()
            pre_st.close()
    return nc, dt_in, tap_aps


def host_constants():
    c = {}
    c["c_identf"] = np.eye(128, dtype=np.float32)
    c["c_identb"] = np.eye(128, dtype=np.float32).astype(ml_dtypes.bfloat16)
    half = 16
    inv = (10000.0 ** (-np.arange(half, dtype=np.float32) / half)).astype(np.float32)
    ang = (np.arange(T, dtype=np.float32)[None, :] * inv[:, None]).astype(np.float32)
    cs = np.cos(ang).astype(np.float32)
    sn = np.sin(ang).astype(np.float32)
    c["c_cos"] = np.concatenate([cs, cs], axis=0)
    c["c_sin"] = np.concatenate([-sn, sn], axis=0)
    s_idx = np.arange(128)
    c["c_tri"] = (s_idx[:, None] <= s_idx[None, :]).astype(np.float32).astype(ml_dtypes.bfloat16)
    c["c_triadd"] = np.where(s_idx[None, :] <= s_idx[:, None], 0.0, -BIG).astype(np.float32)
    t = np.arange(T)
    alq = np.stack([-(64.0 * (t // 64)), -(t % 64).astype(np.float64), np.ones(T), np.ones(T)]).astype(np.float32)
    alk = np.stack([np.ones(T), np.ones(T), 64.0 * (t // 64), (t % 64).astype(np.float64)]).astype(np.float32)
    c["c_alq"] = alq.astype(ml_dtypes.bfloat16)
    c["c_alk"] = alk.astype(ml_dtypes.bfloat16)
    return c


def make_in_maps(inputs, n_cores=8):
    consts = host_constants()
    maps = []
    for b in range(n_cores):
        m = {"x": np.ascontiguousarray(inputs["x"][b], dtype=np.float32)}
        for k, v in inputs.items():
            if k == "x":
                continue
            a = np.asarray(v, dtype=np.float32)
            if k == "router_bias":
                a = a.reshape(1, NE)
            m[k] = np.ascontiguousarray(a)
        m.update(consts)
        maps.append(m)
    return maps


def kernel(**inputs):
    nc, _, _ = build_program()
    maps = make_in_maps(inputs)
    res = run_bass_kernel_spmd(nc, maps, core_ids=list(range(8)))
    return np.stack([np.asarray(r["out"], dtype=np.float32) for r in res.results], axis=0)
```
